# Optimizing a Trainium2 kernel written in Bass

```python
import jax, jax.numpy as jnp
from jax import lax
import numpy as np

D_MODEL = 2048
BATCH = 8
SEQ = 2048
DEPTH = 4

BLOCK_Q = 128
HEAD_DIM = 128
ROPE_THETA = 10000.0
EPS = 1e-6
N_BRANCH = 4
BRANCH_WIDTH = 512
SB_HEADS = 4
SB_W = SB_HEADS * HEAD_DIM
MLA_HEADS = 4
MLA_NOPE = 128
MLA_ROPE = 64
MLA_V = 128
MLA_KV_RANK = 512
MLA_Q_W = MLA_HEADS * (MLA_NOPE + MLA_ROPE)
RET_HEADS = 4
RET_DK = 128
RET_DV = 128
RET_CHUNK = 128
RET_W = RET_HEADS * RET_DK
RET_VW = RET_HEADS * RET_DV
LRU_WIDTH = 512
LRU_BLOCKS = 4
LRU_BLOCK = LRU_WIDTH // LRU_BLOCKS
CONV_WIDTH = 4
LRU_C = 8.0
D_FF = -(-8 * D_MODEL // (3 * 256)) * 256
SPLIT_SIZES = (SB_W, SB_W, SB_W,
               MLA_Q_W, MLA_KV_RANK, MLA_ROPE,
               RET_W, RET_W, RET_VW, RET_VW,
               LRU_WIDTH, LRU_WIDTH)
IN_WIDTH = sum(SPLIT_SIZES)

kernel_name = "hybrid_gated_sb_mla_ret_rglru_block"


def rmsnorm(x, g):
    xf = x.astype(jnp.float32)
    y = xf * lax.rsqrt(jnp.mean(xf * xf, axis=-1, keepdims=True) + EPS)
    return (y * g.astype(jnp.float32)).astype(x.dtype)


def rope(x, pos):
    d = x.shape[-1]
    freq = ROPE_THETA ** (-jnp.arange(0, d, 2, dtype=jnp.float32) / d)
    ang = pos.astype(jnp.float32)[:, :, None] * freq
    cos = jnp.cos(ang)[:, :, None, :]
    sin = jnp.sin(ang)[:, :, None, :]
    xf = x.astype(jnp.float32)
    x1, x2 = xf[..., : d // 2], xf[..., d // 2:]
    return jnp.concatenate([x1 * cos - x2 * sin, x2 * cos + x1 * sin], axis=-1).astype(x.dtype)


def stick_breaking_attention(q, k, v):
    S, D = q.shape[1], q.shape[-1]
    scale = D ** -0.5
    outs = []
    for i0 in range(0, S, BLOCK_Q):
        L = i0 + BLOCK_Q
        z = jnp.einsum('bqhd,bkhd->bhqk', q[:, i0:L], k[:, :L]).astype(jnp.float32) * scale
        t_idx = i0 + jnp.arange(BLOCK_Q)[:, None]
        s_idx = jnp.arange(L)[None, :]
        mask = s_idx < t_idx
        log_1m_beta = jnp.where(mask, jax.nn.log_sigmoid(-z), 0.0)
        tail = lax.cumsum(log_1m_beta, axis=3, reverse=True) - log_1m_beta
        w = jnp.where(mask, jnp.exp(jax.nn.log_sigmoid(z) + tail), 0.0)
        outs.append(jnp.einsum('bhqk,bkhd->bqhd', w.astype(v.dtype), v[:, :L]))
    return jnp.concatenate(outs, axis=1)


def latent_attention(q, c_kv, k_rope, pos, kv_gain, w_ukv):
    B, S = q.shape[0], q.shape[1]
    q_nope = q[..., :MLA_NOPE]
    q_pe = rope(q[..., MLA_NOPE:], pos)
    k_pe = rope(k_rope[:, :, None, :], pos)[:, :, 0, :]
    kv = (rmsnorm(c_kv, kv_gain) @ w_ukv).reshape(B, S, MLA_HEADS, MLA_NOPE + MLA_V)
    k_nope, v = kv[..., :MLA_NOPE], kv[..., MLA_NOPE:]
    scale = (MLA_NOPE + MLA_ROPE) ** -0.5
    outs = []
    for i0 in range(0, S, BLOCK_Q):
        L = i0 + BLOCK_Q
        s = (jnp.einsum('bqhd,bkhd->bhqk', q_nope[:, i0:L], k_nope[:, :L])
             + jnp.einsum('bqhd,bkd->bhqk', q_pe[:, i0:L], k_pe[:, :L])).astype(jnp.float32) * scale
        mask = jnp.arange(L)[None, :] <= (i0 + jnp.arange(BLOCK_Q))[:, None]
        p = jax.nn.softmax(jnp.where(mask, s, -jnp.inf), axis=-1)
        outs.append(jnp.einsum('bhqk,bkhd->bqhd', p.astype(v.dtype), v[:, :L]))
    return jnp.concatenate(outs, axis=1).reshape(B, S, MLA_HEADS * MLA_V)


def retention(q, k, v, g, pos):
    B, S, H, DK = q.shape
    DV = v.shape[-1]
    C = RET_CHUNK
    N = S // C
    q = rope(q, pos).astype(jnp.float32)
    k = rope(k, pos).astype(jnp.float32) * DK ** -0.5
    log_gamma = jnp.log1p(-(2.0 ** (-5.0 - jnp.arange(H, dtype=jnp.float32))))
    qc = q.reshape(B, N, C, H, DK)
    kc = k.reshape(B, N, C, H, DK)
    vc = v.astype(jnp.float32).reshape(B, N, C, H, DV)
    idx = jnp.arange(C, dtype=jnp.float32)
    diff = idx[:, None] - idx[None, :]
    decay = jnp.where(diff >= 0, jnp.exp(log_gamma[:, None, None] * jnp.maximum(diff, 0.0)), 0.0)
    scores = jnp.einsum('bnqhd,bnkhd->bnhqk', qc, kc) * decay
    inner = jnp.einsum('bnhqk,bnkhe->bnqhe', scores, vc)
    k_decay = jnp.exp(log_gamma[:, None] * (C - 1.0 - idx)[None, :])
    chunk_kv = jnp.einsum('bnkhd,hk,bnkhe->bnhde', kc, k_decay, vc)
    chunk_decay = jnp.exp(log_gamma * C)[None, :, None, None]

    def step(state, kv_i):
        return state * chunk_decay + kv_i, state

    init = jnp.zeros((B, H, DK, DV), jnp.float32)
    _, prev = lax.scan(step, init, jnp.moveaxis(chunk_kv, 1, 0))
    prev = jnp.moveaxis(prev, 0, 1)
    q_decay = jnp.exp(log_gamma[:, None] * (idx + 1.0)[None, :])
    cross = jnp.einsum('bnqhd,hq,bnhde->bnqhe', qc, q_decay, prev)
    o = (inner + cross).reshape(B, S, H, DV)
    o = o * lax.rsqrt(jnp.mean(o * o, axis=-1, keepdims=True) + EPS)
    o = o.reshape(B, S, H * DV) * jax.nn.silu(g.astype(jnp.float32))
    return o.astype(v.dtype)


def rglru_block(x_in, y_in, conv_w, conv_b, w_a, b_a, w_x, b_x, lam):
    B, S, W = x_in.shape
    xf = x_in.astype(jnp.float32)
    xp = jnp.pad(xf, ((0, 0), (CONV_WIDTH - 1, 0), (0, 0)))
    xc = conv_b.astype(jnp.float32) + xp[:, 0:S] * conv_w[0]
    for tap in range(1, CONV_WIDTH):
        xc = xc + xp[:, tap:tap + S] * conv_w[tap]
    xb = xc.reshape(B, S, LRU_BLOCKS, LRU_BLOCK)
    r = jax.nn.sigmoid(jnp.einsum('bshi,hij->bshj', xb, w_a.astype(jnp.float32)).reshape(B, S, W) + b_a)
    i = jax.nn.sigmoid(jnp.einsum('bshi,hij->bshj', xb, w_x.astype(jnp.float32)).reshape(B, S, W) + b_x)
    log_a = -LRU_C * r * jax.nn.softplus(-lam.astype(jnp.float32))
    a = jnp.exp(log_a)
    b = jnp.sqrt(-jnp.expm1(2.0 * log_a)) * (i * xc)

    def combine(left, right):
        a1, b1 = left
        a2, b2 = right
        return a1 * a2, a2 * b1 + b2

    _, h = lax.associative_scan(combine, (a, b), axis=1)
    return (h * jax.nn.gelu(y_in.astype(jnp.float32))).astype(x_in.dtype)


def hybrid_layer(x, positions, ln1, w_in, kv_gain, w_ukv, conv_w, conv_b, w_a, b_a, w_x, b_x, lam,
                 w_branch, w_gate, w_out, ln2, wf_gate, wf_up, wf_down):
    B, S, _ = x.shape
    h = rmsnorm(x, ln1)
    proj = h @ w_in
    bounds = np.cumsum(SPLIT_SIZES)[:-1].tolist()
    (sb_q, sb_k, sb_v, mla_q, mla_ckv, mla_kr,
     ret_q, ret_k, ret_v, ret_g, lru_x, lru_y) = jnp.split(proj, bounds, axis=-1)

    def heads(t, n):
        return t.reshape(B, S, n, -1)

    o_sb = stick_breaking_attention(heads(sb_q, SB_HEADS), heads(sb_k, SB_HEADS),
                                    heads(sb_v, SB_HEADS)).reshape(B, S, SB_W)
    o_mla = latent_attention(heads(mla_q, MLA_HEADS), mla_ckv, mla_kr, positions, kv_gain, w_ukv)
    o_ret = retention(heads(ret_q, RET_HEADS), heads(ret_k, RET_HEADS), heads(ret_v, RET_HEADS),
                      ret_g, positions)
    o_lru = rglru_block(lru_x, lru_y, conv_w, conv_b, w_a, b_a, w_x, b_x, lam)
    branches = (o_sb, o_mla, o_ret, o_lru)

    mixed = jax.nn.sigmoid((h @ w_gate[0]).astype(jnp.float32)) * (branches[0] @ w_branch[0])
    for br in range(1, N_BRANCH):
        gate = jax.nn.sigmoid((h @ w_gate[br]).astype(jnp.float32))
        mixed = mixed + gate * (branches[br] @ w_branch[br])
    x = x + mixed.astype(x.dtype) @ w_out

    h2 = rmsnorm(x, ln2)
    x = x + (jax.nn.silu(h2 @ wf_gate) * (h2 @ wf_up)) @ wf_down
    return x


def setup_inputs(seed: int = 0) -> dict:
    key = jax.random.key(seed)
    ks = jax.random.split(key, 24)
    f32 = jnp.float32
    D = D_MODEL

    def nrm(k, shape, fan_in):
        return jax.random.normal(k, shape, f32) * fan_in ** -0.5

    def gain(k, shape):
        return 1.0 + 0.05 * jax.random.normal(k, shape, f32)

    u = jax.random.uniform(ks[10], (DEPTH, LRU_WIDTH), f32, minval=0.9, maxval=0.999)
    s = u ** (1.0 / LRU_C)
    lam = jnp.log(s) - jnp.log1p(-s)
    return {
        "x": jax.random.normal(ks[0], (BATCH, SEQ, D), f32),
        "positions": jnp.tile(jnp.arange(SEQ, dtype=jnp.int32)[None, :], (BATCH, 1)),
        "ln1": gain(ks[1], (DEPTH, D)),
        "w_in": nrm(ks[2], (DEPTH, D, IN_WIDTH), D),
        "mla_kv_gain": gain(ks[3], (DEPTH, MLA_KV_RANK)),
        "mla_w_ukv": nrm(ks[4], (DEPTH, MLA_KV_RANK, MLA_HEADS * (MLA_NOPE + MLA_V)), MLA_KV_RANK),
        "lru_conv_w": nrm(ks[5], (DEPTH, CONV_WIDTH, LRU_WIDTH), CONV_WIDTH),
        "lru_conv_b": 0.01 * jax.random.normal(ks[6], (DEPTH, LRU_WIDTH), f32),
        "lru_w_a": nrm(ks[7], (DEPTH, LRU_BLOCKS, LRU_BLOCK, LRU_BLOCK), LRU_BLOCK),
        "lru_b_a": 0.01 * jax.random.normal(ks[8], (DEPTH, LRU_WIDTH), f32),
        "lru_w_x": nrm(ks[9], (DEPTH, LRU_BLOCKS, LRU_BLOCK, LRU_BLOCK), LRU_BLOCK),
        "lru_b_x": 0.01 * jax.random.normal(ks[11], (DEPTH, LRU_WIDTH), f32),
        "lru_lambda": lam,
        "w_branch": nrm(ks[12], (DEPTH, N_BRANCH, BRANCH_WIDTH, D), BRANCH_WIDTH),
        "w_gate": nrm(ks[13], (DEPTH, N_BRANCH, D, D), D),
        "w_out": nrm(ks[14], (DEPTH, D, D), D),
        "ln2": gain(ks[15], (DEPTH, D)),
        "ffn_w_gate": nrm(ks[16], (DEPTH, D, D_FF), D),
        "ffn_w_up": nrm(ks[17], (DEPTH, D, D_FF), D),
        "ffn_w_down": nrm(ks[18], (DEPTH, D_FF, D), D_FF),
        "ln_final": gain(ks[19], (D,)),
    }


def reference(x, positions, ln1, w_in, mla_kv_gain, mla_w_ukv, lru_conv_w, lru_conv_b, lru_w_a,
              lru_b_a, lru_w_x, lru_b_x, lru_lambda, w_branch, w_gate, w_out, ln2,
              ffn_w_gate, ffn_w_up, ffn_w_down, ln_final):
    for layer in range(DEPTH):
        x = hybrid_layer(x, positions, ln1[layer], w_in[layer], mla_kv_gain[layer], mla_w_ukv[layer],
                         lru_conv_w[layer], lru_conv_b[layer], lru_w_a[layer], lru_b_a[layer],
                         lru_w_x[layer], lru_b_x[layer], lru_lambda[layer], w_branch[layer],
                         w_gate[layer], w_out[layer], ln2[layer], ffn_w_gate[layer],
                         ffn_w_up[layer], ffn_w_down[layer])
    return rmsnorm(x, ln_final)
```

```python
import math
from contextlib import ExitStack

import numpy as np
import concourse.bass as bass
import concourse.mybir as mybir
from concourse.bass_utils import run_bass_kernel_spmd

F32 = mybir.dt.float32
BF16 = mybir.dt.bfloat16
I32 = mybir.dt.int32
AF = mybir.ActivationFunctionType
ALU = mybir.AluOpType
AX = mybir.AxisListType

T = 2048
D = 2048
KC = 16
NT = 16
DFF = 5632
FC = 44
DEPTH = 4
EPS = 1e-6
THETA = 10000.0
TWO_PI = 2.0 * math.pi


class Dep:
    __slots__ = ("sem", "val", "eng")

    def __init__(self, sem, val, eng):
        self.sem = sem
        self.val = val
        self.eng = eng


class Buf:
    __slots__ = ("name", "w", "r")

    def __init__(self, name):
        self.name = name
        self.w = None
        self.r = {}


class DSem:
    __slots__ = ("sem", "cnt", "name")

    def __init__(self, sem, name):
        self.sem = sem
        self.cnt = 0
        self.name = name


class Eng:
    def __init__(self, name, h, is_pe=False):
        self.name = name
        self.h = h
        self.is_pe = is_pe
        self.sem = None
        self.cnt = 0
        self.pending = False
        self.known = {}


class Sy:
    EPOCH = 30000

    def __init__(self, nc, stack):
        self.nc = nc
        self.stack = stack
        self.nsem = 0
        self.E = {
            "pe": Eng("pe", nc.tensor, True),
            "dve": Eng("dve", nc.vector),
            "act": Eng("act", nc.scalar),
            "pool": Eng("pool", nc.gpsimd),
            "sp": Eng("sp", nc.sync),
        }
        for e in self.E.values():
            e.sem = self.new_sem(e.name)
        self.dsems = []
        self.ninst = 0
        self.trace = {k: [] for k in self.E}
        self.port = Buf("psum_port")
        self.hook = None
        self._inhook = False

    def new_sem(self, name):
        self.nsem += 1
        return self.stack.enter_context(self.nc.semaphore("s%d_%s" % (self.nsem, name)))

    def dsem(self, name):
        d = DSem(self.new_sem(name), name)
        self.dsems.append(d)
        return d

    def _deps(self, reads, writes):
        deps = []
        for b in reads:
            if b.w is not None:
                deps.append((b.w, "raw"))
        for b in writes:
            if b.w is not None:
                deps.append((b.w, "waw"))
            for d in b.r.values():
                deps.append((d, "war"))
        return deps

    def _wait(self, E, deps, is_dma):
        waits = {}
        for d, kind in deps:
            if (not is_dma) and d.eng == E.name:
                if kind != "raw" or E.is_pe:
                    continue
            if E.known.get(d.sem, 0) >= d.val:
                continue
            if waits.get(d.sem, (None, 0))[1] < d.val:
                waits[d.sem] = (d.sem, d.val)
        for sem, val in waits.values():
            E.h.wait_ge(sem, val)
            E.known[sem] = val
            self.ninst += 1
            self.trace[E.name].append(("w", sem.num, val))

    def op(self, en, emit, reads=(), writes=(), inc=True):
        E = self.E[en]
        if en in ("act", "dve") and any(bb.name.startswith("ps") for bb in reads):
            writes = list(writes) + [self.port]
        self._wait(E, self._deps(reads, writes), False)
        ins = emit(E.h)
        self.ninst += 1
        if inc:
            if E.cnt >= self.EPOCH and not E.pending:
                E.sem = self.new_sem(E.name)
                E.cnt = 0
            E.cnt += 1
            ins.then_inc(E.sem, 1)
            self.trace[en].append(("i", E.sem.num, 1))
            E.pending = False
            d = Dep(E.sem, E.cnt, en)
        else:
            E.pending = True
            d = Dep(E.sem, E.cnt + 1, en)
        for b in reads:
            b.r[en] = d
        for b in writes:
            b.w = d
            b.r = {}
        if self.hook is not None and not self._inhook and en in ("dve", "act"):
            self._inhook = True
            self.hook()
            self._inhook = False
        return ins

    def dma(self, qn, ds, out_ap, in_ap, reads=(), writes=()):
        E = self.E[qn]
        self._wait(E, self._deps(reads, writes), True)
        ins = E.h.dma_start(out=out_ap, in_=in_ap)
        self.ninst += 1
        ds.cnt += 16
        ins.then_inc(ds.sem, 16)
        self.trace[qn].append(("i", ds.sem.num, 16))
        d = Dep(ds.sem, ds.cnt, "dma")
        for b in reads:
            b.r["dma:" + ds.name] = d
        for b in writes:
            b.w = d
            b.r = {}
        return ins

    def barrier(self, only=None, skip=("pool",)):
        targets = []
        for e in self.E.values():
            assert not e.pending, "pending instruction on %s at barrier" % e.name
            if e.cnt > 0:
                targets.append((e.name, e.sem, e.cnt))
        for ds in self.dsems:
            if ds.cnt > 0:
                targets.append(("dma", ds.sem, ds.cnt))
        for e in self.E.values():
            if only is not None and e.name not in only:
                continue
            if only is None and e.name in skip:
                continue
            for (src, sem, val) in targets:
                if src == e.name:
                    continue
                if e.known.get(sem, 0) >= val:
                    continue
                e.h.wait_ge(sem, val)
                e.known[sem] = val
                self.ninst += 1
                self.trace[e.name].append(("w", sem.num, val))

    def check_deadlock(self):
        val = {}
        ptr = {k: 0 for k in self.trace}
        progress = True
        while progress:
            progress = False
            for k, tr in self.trace.items():
                while ptr[k] < len(tr):
                    kind, sname, v = tr[ptr[k]]
                    if kind == "w":
                        if val.get(sname, 0) >= v:
                            ptr[k] += 1
                            progress = True
                        else:
                            break
                    else:
                        val[sname] = val.get(sname, 0) + v
                        ptr[k] += 1
                        progress = True
        stuck = {k: (ptr[k], len(tr), tr[ptr[k]], val.get(tr[ptr[k]][1], 0)) for k, tr in self.trace.items() if ptr[k] < len(tr)}
        return stuck


C_SBQ, C_SBK, C_SBV = 0, 512, 1024
C_MQ, C_CKV, C_KR = 1536, 2304, 2816
C_RQ, C_RK, C_RV, C_RG = 2880, 3392, 3904, 4416
C_LX, C_LY = 4928, 5440


def _win_jobs():
    jobs = []
    ar = np.arange
    jobs.append(("sbv0", ar(C_SBV, C_SBV + 256)))
    jobs.append(("sbv1", ar(C_SBV + 256, C_SBV + 512)))
    for h in range(4):
        jobs.append(("sbqk%d" % h, np.concatenate([ar(C_SBQ + 128 * h, C_SBQ + 128 * h + 128),
                                                   ar(C_SBK + 128 * h, C_SBK + 128 * h + 128)])))
    jobs.append(("ckv0", ar(C_CKV, C_CKV + 256)))
    jobs.append(("ckv1", ar(C_CKV + 256, C_CKV + 512)))
    jobs.append(("kr", ar(C_KR, C_KR + 64)))
    for h in range(4):
        jobs.append(("mq%d" % h, ar(C_MQ + 192 * h, C_MQ + 192 * h + 192)))
    jobs.append(("rv0", ar(C_RV, C_RV + 256)))
    jobs.append(("rv1", ar(C_RV + 256, C_RV + 512)))
    jobs.append(("rg0", ar(C_RG, C_RG + 256)))
    jobs.append(("rg1", ar(C_RG + 256, C_RG + 512)))
    for h in range(4):
        jobs.append(("rqk%d" % h, np.concatenate([ar(C_RQ + 128 * h, C_RQ + 128 * h + 128),
                                                  ar(C_RK + 128 * h, C_RK + 128 * h + 128)])))
    for c in range(4):
        jobs.append(("lxy%d" % c, np.concatenate([ar(C_LX + 128 * c, C_LX + 128 * c + 128),
                                                  ar(C_LY + 128 * c, C_LY + 128 * c + 128)])))
    return jobs


WIN_JOBS = _win_jobs()
WIN_OFF = {}
_o = 0
for _n, _c in WIN_JOBS:
    WIN_OFF[_n] = (_o, len(_c))
    _o += KC * len(_c)
WIN_TOT = _o


def _pack_cols(W, cols):
    K = W.shape[0]
    sub = W[:, cols]
    return np.ascontiguousarray(sub.reshape(K // 128, 128, len(cols)).transpose(1, 0, 2)).reshape(128, -1)


def _tile_cols(W, tw):
    K, Fd = W.shape
    return np.ascontiguousarray(W.reshape(K // 128, 128, Fd // tw, tw).transpose(2, 1, 0, 3)).reshape(Fd // tw, 128, -1)


def _ukv_pack(Wukv):
    blocks = []
    for h in range(4):
        blocks.append(_pack_cols(Wukv, np.arange(h * 256, h * 256 + 128)))
    vcols = np.concatenate([np.arange(h * 256 + 128, h * 256 + 256) for h in range(4)])
    blocks.append(_pack_cols(Wukv, vcols[:256]))
    blocks.append(_pack_cols(Wukv, vcols[256:]))
    return np.concatenate(blocks, axis=1)


PV_LN1, PV_LN2, PV_KVG, PV_CW, PV_CB, PV_BA, PV_BX, PV_LAM = 0, 16, 32, 36, 52, 56, 60, 64
NPV = 68

CC_FM, CC_FR, CC_KDEC, CC_ONE, CC_EPS = 0, 1, 2, 6, 7
NCC = 8
CM_ID, CM_R64, CM_R128, CM_LT, CM_NEG, CM_DEC, CM_QDEC = 0, 1, 2, 3, 4, 5, 9
NCM = 13


def _consts():
    cc = np.zeros((128, NCC), np.float32)
    p = np.arange(128)
    cc[:, CC_FM] = (THETA ** (-(2.0 * (p % 32)) / 64.0)).astype(np.float32)
    cc[:, CC_FR] = (THETA ** (-(2.0 * (p % 64)) / 128.0)).astype(np.float32)
    gam = 1.0 - 2.0 ** (-5.0 - np.arange(4, dtype=np.float64))
    lg = np.log(gam)
    for h in range(4):
        cc[:, CC_KDEC + h] = np.exp(lg[h] * (127.0 - p)).astype(np.float32)
    cc[:, CC_ONE] = 1.0
    cc[:, CC_EPS] = EPS
    cm = np.zeros((NCM, 128, 128), np.float32)
    cm[CM_ID] = np.eye(128, dtype=np.float32)
    for m in range(64):
        if m < 32:
            cm[CM_R64, m + 32, m] = -1.0
        else:
            cm[CM_R64, m - 32, m] = 1.0
    for m in range(128):
        if m < 64:
            cm[CM_R128, m + 64, m] = -1.0
        else:
            cm[CM_R128, m - 64, m] = 1.0
    tt = np.arange(128)[:, None]
    ss = np.arange(128)[None, :]
    cm[CM_LT] = (ss < tt).astype(np.float32)
    cm[CM_NEG] = np.where(ss <= tt, 0.0, -1.0e30).astype(np.float32)
    for h in range(4):
        dq = (ss - tt).astype(np.float64)
        cm[CM_DEC + h] = np.where(dq >= 0, np.exp(lg[h] * np.maximum(dq, 0.0)), 0.0).astype(np.float32)
        cm[CM_QDEC + h] = np.broadcast_to(np.exp(lg[h] * (np.arange(128) + 1.0))[None, :], (128, 128)).astype(np.float32)
    cd = [float(np.exp(lg[h] * 128.0)) for h in range(4)]
    return cc, cm, cd


def _col128(v):
    return np.ascontiguousarray(v.reshape(-1, 128).T)


def prep_weights(inp, layers):
    out = {}
    NL = len(layers)
    winp = np.empty((NL, 128, WIN_TOT), np.float32)
    ukv = np.empty((NL, 128, 4096), np.float32)
    wax = np.empty((NL, 128, 1024), np.float32)
    wbr = np.empty((NL, 16, 128, 2048), np.float32)
    wgt = np.empty((NL, 64, 128, 2048), np.float32)
    wout = np.empty((NL, 16, 128, 2048), np.float32)
    wfgu = np.empty((NL, FC, 128, 4096), np.float32)
    wfd = np.empty((NL, 16, 128, FC * 128), np.float32)
    pv = np.zeros((NL, 128, NPV), np.float32)
    for i, l in enumerate(layers):
        W = inp["w_in"][l]
        for n, cols in WIN_JOBS:
            off, nc_ = WIN_OFF[n]
            winp[i, :, off:off + KC * nc_] = _pack_cols(W, cols)
        ukv[i] = _ukv_pack(inp["mla_w_ukv"][l])
        wa = inp["lru_w_a"][l]
        wx = inp["lru_w_x"][l]
        wax[i, :, 0:512] = wa.transpose(1, 0, 2).reshape(128, 512)
        wax[i, :, 512:1024] = wx.transpose(1, 0, 2).reshape(128, 512)
        wb = inp["w_branch"][l].reshape(4, 4, 128, 16, 128)
        wbr[i] = wb.transpose(3, 2, 0, 1, 4).reshape(16, 128, 2048)
        wg = inp["w_gate"][l].reshape(4, 16, 128, 16, 128)
        wgt[i] = wg.transpose(3, 0, 2, 1, 4).reshape(64, 128, 2048)
        wout[i] = _tile_cols(inp["w_out"][l], 128)
        g = _tile_cols(inp["ffn_w_gate"][l], 128)
        u = _tile_cols(inp["ffn_w_up"][l], 128)
        wfgu[i, :, :, 0:2048] = g
        wfgu[i, :, :, 2048:4096] = u
        wfd[i] = _tile_cols(inp["ffn_w_down"][l], 128)
        pv[i, :, PV_LN1:PV_LN1 + 16] = _col128(inp["ln1"][l])
        pv[i, :, PV_LN2:PV_LN2 + 16] = _col128(inp["ln2"][l])
        pv[i, :, PV_KVG:PV_KVG + 4] = _col128(inp["mla_kv_gain"][l])
        cw = inp["lru_conv_w"][l]
        for tap in range(4):
            pv[i, :, PV_CW + 4 * tap:PV_CW + 4 * tap + 4] = _col128(cw[tap])
        pv[i, :, PV_CB:PV_CB + 4] = _col128(inp["lru_conv_b"][l])
        pv[i, :, PV_BA:PV_BA + 4] = _col128(inp["lru_b_a"][l])
        pv[i, :, PV_BX:PV_BX + 4] = _col128(inp["lru_b_x"][l])
        pv[i, :, PV_LAM:PV_LAM + 4] = _col128(inp["lru_lambda"][l])
    out.update(winp=winp, ukv=ukv, wax=wax, wbr=wbr, wgt=wgt, wout=wout, wfgu=wfgu, wfd=wfd, pv=pv)
    return out


def build(NL, final_norm=True, dbg=False, stages=("mix", "A", "B", "C", "D", "gate", "wout", "ffn")):
    nc = bass.Bass("TRN2", target_bir_lowering=False)
    cc_np, cm_np, CD = _consts()

    def din(name, shape, dt=F32):
        return nc.dram_tensor(name, list(shape), dt, kind="ExternalInput").ap()

    xT_in = din("xT", [D, T])
    pos_in = din("pos", [1, T], I32)
    lnf_in = din("lnf", [128, 16])
    cc_in = din("cc", [128, NCC])
    cm_in = din("cm", [NCM, 128, 128])
    winp = din("winp", [NL, 128, WIN_TOT])
    ukv = din("ukv", [NL, 128, 4096])
    wax = din("wax", [NL, 128, 1024])
    wbr = din("wbr", [NL, 16, 128, 2048])
    wgt = din("wgt", [NL, 64, 128, 2048])
    wout = din("wout", [NL, 16, 128, 2048])
    wfgu = din("wfgu", [NL, FC, 128, 4096])
    wfd = din("wfd", [NL, 16, 128, FC * 128])
    pv_in = din("pv", [NL, 128, NPV])
    outT = nc.dram_tensor("outT", [D, T], F32, kind="ExternalOutput").ap()
    skind = "ExternalOutput" if dbg else "Internal"
    xres = nc.dram_tensor("xres", [D, T], F32, kind=skind).ap()
    obT = nc.dram_tensor("obT", [2048, T], BF16, kind=skind).ap()
    mixT = nc.dram_tensor("mixT", [D, T], BF16, kind=skind).ap()
    sgD = nc.dram_tensor("sgD", [4, D, T], BF16, kind="Internal").ap()

    with ExitStack() as st:
        S = Sy(nc, st)

        uid = [0]

        def sb(name, shape, dt, stack=st):
            uid[0] += 1
            return stack.enter_context(nc.sbuf_tensor("%s_u%d" % (name, uid[0]), list(shape), dt))

        hbuf = sb("hbuf", [128, KC, T], BF16)
        Bh = Buf("hbuf")
        WSL = 6144
        wslots = []
        for i in range(3):
            wslots.append((sb("wslot%d" % i, [128, WSL], BF16), Buf("wslot%d" % i), S.dsem("wld%d" % i)))
        wctr = [0]
        ccs = sb("ccs", [128, NCC], F32)
        pvs = sb("pvs", [128, NPV], F32)
        lnfs = sb("lnfs", [128, 16], F32)
        Bcc, Bpv = Buf("ccs"), Buf("pvs")
        ident_f = sb("ident_f", [128, 128], F32)
        cmb = sb("cmb", [128, 5, 128], BF16)
        cmf = sb("cmf", [128, 9, 128], F32)
        Bcm = Buf("cm")
        ones_bf = sb("ones_bf", [128, 128], BF16)
        Bones = Buf("ones")
        Btab = Buf("tabs")
        ps = [st.enter_context(nc.psum_tensor("ps%d" % i, [128, 512], F32)) for i in range(8)]
        PB = [Buf("ps%d" % i) for i in range(8)]
        lin_ctr = [0]
        lin_nb = [4]
        ev_ctr = [0]
        misc_ds = S.dsem("misc")
        out_ds = S.dsem("outst")
        ODS = [S.dsem("ost%d" % i) for i in range(2)]
        GWD = [S.dsem("gwl%d" % i) for i in range(3)]
        GWB = [Buf("gw%d" % i) for i in range(3)]
        SGD = [S.dsem("sgst%d" % i) for i in range(4)]
        sgs = [(sb("sgs%d" % i, [128, 512], BF16), Buf("sgs%d" % i)) for i in range(4)]
        gbanks = [[0, 1, 2, 3]]
        XDS = [S.dsem("oxl%d" % i) for i in range(2)]
        FDS = [S.dsem("fxl%d" % i) for i in range(2)]
        NDS = [S.dsem("nxl%d" % i) for i in range(3)]
        NSD = [S.dsem("nxs%d" % i) for i in range(3)]
        WBDS = [S.dsem("wbs%d" % i) for i in range(4)]
        tabD = nc.dram_tensor("tabD", [4, 128, T], BF16, kind="Internal").ap()

        ident_bf = cmb[:, 0, :]
        r64_bf = cmb[:, 1, :]
        r128_bf = cmb[:, 2, :]
        lt_bf = cmb[:, 3, :]

        def lt_f():
            return cmf_lt[:]

        wl_n = [3]

        def wload(src2d, nelem):
            i = wctr[0] % wl_n[0]
            wctr[0] += 1
            t, b, ds = wslots[i]
            S.dma("pool", ds, t[:, 0:nelem], src2d, writes=[b])
            return t, b

        def lin_bank(n=None):
            n = lin_nb[0] if n is None else n
            b = lin_ctr[0] % n
            lin_ctr[0] += 1
            return b

        def evac(out_ap, in_ap, reads, writes, scale=None):
            use_act = (ev_ctr[0] % 2 == 0)
            ev_ctr[0] += 1
            import os
            if "evact" in os.environ.get("K_DBG", ""):
                use_act = True
            if "evdve" in os.environ.get("K_DBG", ""):
                use_act = False
            if use_act:
                if scale is None:
                    S.op("act", lambda e: e.activation(out_ap, in_ap, AF.Copy), reads, writes)
                else:
                    S.op("act", lambda e: e.activation(out_ap, in_ap, AF.Copy, scale=scale), reads, writes)
            else:
                if scale is None:
                    S.op("dve", lambda e: e.tensor_copy(out_ap, in_ap), reads, writes)
                else:
                    S.op("dve", lambda e: e.tensor_scalar(out_ap, in_ap, scale, None, ALU.mult), reads, writes)

        def mm(out_ap, lhsT, rhs, start, stop, reads, writes, inc=None):
            S.op("pe", lambda e: e.matmul(out_ap, lhsT, rhs, start=start, stop=stop), reads, writes, inc=(stop if inc is None else inc))

        def lin_fm(wt, wb, col0, M, nk, rhs_fn, rhs_bufs, epi, ncols_job, tgs=range(4), N=512):
            wv = wt[:, 0:nk * ncols_job].rearrange("p (c n) -> p c n", n=ncols_job)
            for tg in tgs:
                bk = lin_bank()
                for kc in range(nk):
                    mm(ps[bk][0:M, 0:N], wv[:, kc, col0:col0 + M], rhs_fn(kc, tg), kc == 0, kc == nk - 1,
                       [wb] + rhs_bufs, [PB[bk]])
                epi(tg, bk)

        def lin_tm(wt, wb, col0, Ncol, nk, lhs_fn, lhs_bufs, epi, ncols_job):
            wv = wt[:, 0:nk * ncols_job].rearrange("p (c n) -> p c n", n=ncols_job)
            import os
            dbgm = os.environ.get("K_DBG", "")
            for tile in range(NT):
                bk = lin_bank()
                for kc in range(nk):
                    if "nomm" in dbgm and kc > 0:
                        continue
                    mm(ps[bk][:, 0:Ncol], lhs_fn(kc, tile), wv[:, kc, col0:col0 + Ncol], kc == 0, (kc == nk - 1) or ("nomm" in dbgm),
                       [wb] + lhs_bufs, [PB[bk]])
                if "noevac" not in dbgm:
                    epi(tile, bk)

        def h_rhs(kc, tg):
            return hbuf[:, kc, tg * 512:(tg + 1) * 512]

        def h_lhs(kc, tile):
            return hbuf[:, kc, tile * 128:(tile + 1) * 128]

        S.dma("sp", misc_ds, ccs[:], cc_in, writes=[Bcc])
        S.dma("sp", misc_ds, ident_f[:], cm_in[CM_ID], writes=[Bcm])
        S.dma("sp", misc_ds, lnfs[:], lnf_in, writes=[Bcc])
        cmf_lt = sb("cmf_lt", [128, 128], F32)
        S.dma("sp", misc_ds, cmf_lt[:], cm_in[CM_LT], writes=[Bcm])
        S.dma("sp", misc_ds, cmf[:], cm_in[CM_NEG:CM_NEG + 9].rearrange("n p j -> p n j"), writes=[Bcm])
        S.dma("pool", misc_ds, cmb[:, 0:4, :], cm_in[CM_ID:CM_ID + 4].rearrange("n p j -> p n j"), writes=[Bcm])
        S.op("dve", lambda e: e.memset(ones_bf[:], 1.0), writes=[Bones])
        notlt = sb("notlt", [128, 128], F32)
        S.barrier()

        S.op("dve", lambda e: e.tensor_scalar(notlt[:], cmf_lt[:], -1.0, 1.0, ALU.mult, ALU.add), [Bcm], [Bcm])
        with ExitStack() as ts:
            posi = sb("posi", [128, T], I32, ts)
            posf = sb("posf", [128, T], F32, ts)
            ang = sb("ang", [128, T], F32, ts)
            kf = sb("kf", [128, T], F32, ts)
            ki = sb("ki", [128, T], I32, ts)
            Bt = [Buf("t%d" % i) for i in range(5)]
            pos_b = bass.AP(pos_in.tensor, pos_in.offset, [[0, 128], [1, T]])
            S.dma("sp", misc_ds, posi[:], pos_b, writes=[Bt[0]])
            S.op("dve", lambda e: e.tensor_copy(posf[:], posi[:]), [Bt[0]], [Bt[1]])
            tabs = sb("tabs", [128, T], BF16, ts)
            for (fcol, ci, si) in ((CC_FM, 0, 1), (CC_FR, 2, 3)):
                for shift, ti in ((0.0, si), (0.5 * math.pi, ci)):
                    tab = tabs
                    S.op("dve", lambda e: e.tensor_scalar(ang[:], posf[:], ccs[:, fcol:fcol + 1], None, ALU.mult),
                         [Bt[1], Bcc], [Bt[2]])
                    if shift != 0.0:
                        S.op("dve", lambda e: e.tensor_scalar(ang[:], ang[:], shift, None, ALU.add), [Bt[2]], [Bt[2]])
                    S.op("dve", lambda e: e.tensor_scalar(kf[:], ang[:], 1.0 / TWO_PI, None, ALU.mult), [Bt[2]], [Bt[3]])
                    S.op("dve", lambda e: e.tensor_copy(ki[:], kf[:]), [Bt[3]], [Bt[4]])
                    S.op("dve", lambda e: e.tensor_copy(kf[:], ki[:]), [Bt[4]], [Bt[3]])
                    c1 = 6.28125
                    c2 = TWO_PI - c1
                    S.op("dve", lambda e: e.scalar_tensor_tensor(ang[:], kf[:], -c1, ang[:], ALU.mult, ALU.add), [Bt[3], Bt[2]], [Bt[2]])
                    S.op("dve", lambda e: e.scalar_tensor_tensor(ang[:], kf[:], -c2, ang[:], ALU.mult, ALU.add), [Bt[3], Bt[2]], [Bt[2]])
                    S.op("dve", lambda e: e.tensor_scalar(ang[:], ang[:], math.pi, -math.pi, ALU.min, ALU.max), [Bt[2]], [Bt[2]])
                    S.op("act", lambda e: e.activation(tab[:], ang[:], AF.Sin), [Bt[2]], [Btab])
                    S.dma("sp", misc_ds, tabD[ti], tab[:], reads=[Btab])
            S.barrier()

        def norm_phase(src, gsb, gcol, final):
            G = 256 if final else 512
            nb = 3 if final else 2
            with ExitStack() as ns:
                xb = [sb("nx%d" % i, [128, KC, G], F32, ns) for i in range(nb)]
                Bx = [Buf("nx%d" % i) for i in range(nb)]
                sq = [sb("nsq%d" % i, [128, G], BF16, ns) for i in range(2)]
                Bsq = [Buf("nsq%d" % i) for i in range(2)]
                rsb = sb("nrs", [128, G], F32, ns)
                Brs = Buf("nrs")
                srcv = src.rearrange("(c p) t -> p c t", p=128)
                ngrp = T // G

                def load_grp(g_):
                    for hh in range(4):
                        S.dma("sp", NDS[g_ % nb], xb[g_ % nb][:, hh * 4:(hh + 1) * 4, :], srcv[:, hh * 4:(hh + 1) * 4, g_ * G:(g_ + 1) * G],
                              writes=[Bx[g_ % nb]])
                for g_ in range(min(nb - 1, ngrp)):
                    load_grp(g_)
                for tq in range(ngrp):
                    x_, bx = xb[tq % nb], Bx[tq % nb]
                    tsl = slice(tq * G, (tq + 1) * G)
                    if tq + nb - 1 < ngrp:
                        load_grp(tq + nb - 1)
                    bk = lin_bank()
                    for c in range(KC):
                        s_, bs = sq[c % 2], Bsq[c % 2]
                        S.op("act", lambda e: e.activation(s_[:], x_[:, c, :], AF.Square), [bx], [bs])
                        mm(ps[bk][:, 0:G], ones_bf[:, 0:128], s_[:], c == 0, c == KC - 1, [Bones, bs], [PB[bk]], inc=True)
                    S.op("act", lambda e: e.activation(rsb[:], ps[bk][:, 0:G], AF.Sqrt, bias=ccs[:, CC_EPS:CC_EPS + 1], scale=1.0 / D),
                         [PB[bk], Bcc], [Brs])
                    S.op("dve", lambda e: e.reciprocal(rsb[:], rsb[:]), [Brs], [Brs])
                    for c in range(KC):
                        if final:
                            S.op("dve", lambda e: e.scalar_tensor_tensor(x_[:, c, :], x_[:, c, :], gsb[:, gcol + c:gcol + c + 1], rsb[:],
                                                                          ALU.mult, ALU.mult), [bx, Brs, Bpv, Bcc], [bx])
                        else:
                            S.op("dve", lambda e: e.scalar_tensor_tensor(hbuf[:, c, tsl], x_[:, c, :],
                                                                          gsb[:, gcol + c:gcol + c + 1], rsb[:], ALU.mult, ALU.mult),
                                 [bx, Brs, Bpv, Bcc], [Bh])
                    if final:
                        ov = outT.rearrange("(c p) t -> p c t", p=128)
                        for hh in range(4):
                            S.dma("sp", NSD[tq % nb], ov[:, hh * 4:(hh + 1) * 4, tsl], x_[:, hh * 4:(hh + 1) * 4, :], reads=[bx])
                S.barrier()

        def rope_fm(bk, R, ctab, stab, rmat, scale, dst_ap, dst_buf, tg, tmp):
            xs, t1, t2, Bxs, Bt1, Bt2 = tmp
            sl = slice(tg * 512, (tg + 1) * 512)
            S.op("act", lambda e: e.activation(xs[0:R, :], ps[bk][0:R, :], AF.Copy, scale=scale), [PB[bk]], [Bxs])
            b2 = lin_bank()
            mm(ps[b2][0:R, :], rmat[0:R, 0:R], xs[0:R, :], True, True, [Bcm, Bxs], [PB[b2]])
            S.op("dve", lambda e: e.tensor_tensor(t1[0:R, :], xs[0:R, :], ctab[0:R, sl], ALU.mult), [Bxs, Btab], [Bt1])
            S.op("dve", lambda e: e.tensor_tensor(t2[0:R, :], ps[b2][0:R, :], stab[0:R, sl], ALU.mult), [PB[b2], Btab], [Bt2])
            S.op("dve", lambda e: e.tensor_tensor(dst_ap, t1[0:R, :], t2[0:R, :], ALU.add), [Bt1, Bt2], [dst_buf])

        def attn_core(mode, qT, Bq, kT, Bk, vall, Bv, h, W, oT, Bo, qpe=None, kpe=None, Bpe=None, Bqpe=None, ones_big=None, Bonesb=None, tickf=None):
            e1, zc, Pb, wb_, wT, col, dg, Be1, Bzc, BPb, Bw, BwT, Bcol, Bdg = W
            for i in range(NT if "att1" not in stages else 1):
                L = 128 * (i + 1)
                nch = (L + 511) // 512
                for ch in range(nch):
                    cw = min(512, L - ch * 512)
                    bk = 4 + (ch % 2)
                    cs = slice(ch * 512, ch * 512 + cw)
                    mm(ps[bk][:, 0:cw], qT[:, i * 128:(i + 1) * 128], kT[:, cs], True, mode == "sb", [Bq, Bk], [PB[bk]])
                    if mode == "mla":
                        mm(ps[bk][:, 0:cw], qpe[0:64, i * 128:(i + 1) * 128], kpe[0:64, cs], False, True, [Bqpe, Bpe], [PB[bk]])
                    if mode == "sb":
                        S.op("act", lambda e: e.activation(e1[:, cs], ps[bk][:, 0:cw], AF.Exp), [PB[bk]], [Be1])
                        S.op("dve", lambda e: e.tensor_copy(zc[:, cs], ps[bk][:, 0:cw]), [PB[bk]], [Bzc])
                    else:
                        evac(zc[:, cs], ps[bk][:, 0:cw], [PB[bk]], [Bzc])
                dsl = slice(L - 128, L)
                if tickf is not None:
                    tickf()
                if mode == "sb":
                    S.op("act", lambda e: e.activation(e1[:, 0:L], e1[:, 0:L], AF.Ln, bias=ccs[:, CC_ONE:CC_ONE + 1]), [Be1, Bcc], [Be1])
                    S.op("dve", lambda e: e.tensor_tensor(e1[:, dsl], e1[:, dsl], cmf_lt[:], ALU.mult), [Be1, Bcm], [Be1])
                    S.op("dve", lambda e: e.tensor_tensor_scan(Pb[:, 0:L], ones_big[:, 0:L], e1[:, 0:L], 0.0, ALU.mult, ALU.subtract),
                         [Bonesb, Be1], [BPb])
                    S.op("dve", lambda e: e.tensor_tensor(zc[:, 0:L], zc[:, 0:L], e1[:, 0:L], ALU.subtract), [Bzc, Be1], [Bzc])
                    S.op("dve", lambda e: e.tensor_tensor(zc[:, 0:L], zc[:, 0:L], Pb[:, 0:L], ALU.subtract), [Bzc, BPb], [Bzc])
                    S.op("act", lambda e: e.activation(wb_[:, 0:L], zc[:, 0:L], AF.Exp, bias=Pb[:, L - 1:L]), [Bzc, BPb], [Bw])
                    S.op("dve", lambda e: e.tensor_tensor(wb_[:, dsl], wb_[:, dsl], lt_bf, ALU.mult), [Bw, Bcm], [Bw])
                    rhsT, BrT = ident_bf, Bcm
                else:
                    S.op("dve", lambda e: e.tensor_tensor(zc[:, dsl], zc[:, dsl], cmf[:, 0, :], ALU.add), [Bzc, Bcm], [Bzc])
                    S.op("dve", lambda e: e.tensor_reduce(col[:, 0:1], zc[:, 0:L], AX.X, ALU.max, negate=True), [Bzc], [Bcol])
                    S.op("act", lambda e: e.activation(wb_[:, 0:L], zc[:, 0:L], AF.Exp, bias=col[:, 0:1], accum_out=col[:, 1:2]),
                         [Bzc, Bcol], [Bw, Bcol])
                    S.op("dve", lambda e: e.reciprocal(col[:, 2:3], col[:, 1:2]), [Bcol], [Bcol])
                    S.op("dve", lambda e: e.tensor_scalar(dg[:], ident_f[:], col[:, 2:3], None, ALU.mult), [Bcm, Bcol], [Bdg])
                    rhsT, BrT = dg[:], Bdg
                kb = 0
                while kb <= i:
                    n4 = min(4, i + 1 - kb)
                    for j in range(n4):
                        S.op("pe", lambda e: e.matmul(ps[6][:, j * 128:(j + 1) * 128], wb_[:, (kb + j) * 128:(kb + j + 1) * 128], rhsT,
                                                      start=True, stop=True), [Bw, BrT], [PB[6]], inc=(j == n4 - 1))
                    evac(wT[:, kb * 128:(kb + n4) * 128], ps[6][:, 0:n4 * 128], [PB[6]], [BwT])
                    kb += n4
                for kb in range(i + 1):
                    mm(ps[7][:, 0:128], vall[:, kb, h * 128:(h + 1) * 128], wT[:, kb * 128:(kb + 1) * 128], kb == 0, kb == i,
                       [Bv, BwT], [PB[7]])
                evac(oT[:, i * 128:(i + 1) * 128], ps[7][:, 0:128], [PB[7]], [Bo])

        for l in range(NL):
            S.dma("sp", misc_ds, pvs[:], pv_in[l], writes=[Bpv])

            def wjob(name):
                off, ncj = WIN_OFF[name]
                t, b = wload(winp[l, :, off:off + KC * ncj], KC * ncj)
                return t, b, ncj

            if "mix" in stages:
                norm_phase(xT_in if l == 0 else xres, pvs, PV_LN1, False)
                wl_n[0] = 2
                wctr[0] = 0

                def gate_gen():
                    gt = wslots[2][0]
                    u = 0
                    for m in range(16):
                        for b in range(4):
                            j = (m * 4 + b) % 3
                            gw = gt[:, j * 2048:(j + 1) * 2048]
                            S.dma("pool", GWD[j], gw, wgt[l, m * 4 + b], writes=[GWB[j]])
                            gv = gw.rearrange("p (c j) -> p c j", j=128)
                            for tg in range(4):
                                gb = gbanks[0]
                                gate_flush(len(gb) - 1)
                                bk = gb[u % len(gb)]
                                for kc in range(KC):
                                    mm(ps[bk][:, :], gv[:, kc, :], hbuf[:, kc, tg * 512:(tg + 1) * 512], kc == 0, kc == KC - 1,
                                       [GWB[j], Bh], [PB[bk]])
                                gpend.append((bk, m, b, tg, u))
                                u += 1
                                yield
                    gate_flush()
                    yield

                gpend = []

                def gate_flush(keep=0):
                    while len(gpend) > keep:
                        gate_fin(*gpend.pop(0))

                def gate_fin(bk, m, b, tg, u):
                    st_, Bst = sgs[u % 4]
                    S.op("act", lambda e: e.activation(st_[:], ps[bk][:, :], AF.Sigmoid), [PB[bk]], [Bst])
                    S.dma("sp", SGD[u % 4], sgD[b, m * 128:(m + 1) * 128, tg * 512:(tg + 1) * 512], st_[:], reads=[Bst])

                ggen = gate_gen() if "gate" in stages else iter(())

                def tick(n=1):
                    for _ in range(n):
                        next(ggen, None)

                if "A" in stages:
                    with ExitStack() as bs:
                        vall = sb("a_v", [128, NT, 512], BF16, bs)
                        Bv = Buf("a_v")
                        ctxs = []
                        for c in range(2):
                            cx = dict(
                                qT=sb("a_q%d" % c, [128, T], BF16, bs), Bq=Buf("a_q%d" % c),
                                kT=sb("a_k%d" % c, [128, T], BF16, bs), Bk=Buf("a_k%d" % c),
                                oT=sb("a_o%d" % c, [128, T], BF16, bs), Bo=Buf("a_o%d" % c),
                                bt=sb("a_bt%d" % c, [128, T], BF16, bs), Bbt=Buf("a_bt%d" % c),
                                ob=sb("a_ob%d" % c, [128, T], F32, bs), Bob=Buf("a_ob%d" % c),
                                R=sb("a_R%d" % c, [128, T], BF16, bs), BR=Buf("a_R%d" % c),
                                w=sb("a_w%d" % c, [128, T], BF16, bs), Bw=Buf("a_w%d" % c),
                                wT=sb("a_wT%d" % c, [128, T], BF16, bs), BwT=Buf("a_wT%d" % c),
                                zb=(0, 1) if c == 0 else (4, 5), tb=2 if c == 0 else 6, obk=3 if c == 0 else 7)
                            ctxs.append(cx)
                        for half in range(2):
                            t, b, ncj = wjob("sbv%d" % half)

                            def epi_v(tile, bk, half=half):
                                evac(vall[:, tile, half * 256:(half + 1) * 256], ps[bk][:, 0:256], [PB[bk]], [Bv])
                            lin_tm(t, b, 0, 256, KC, h_lhs, [Bh], epi_v, ncj)

                        def rev(t_, L):
                            ap = t_[:, 0:L]
                            return bass.AP(ap.tensor, ap.offset + (L - 1), [list(ap.ap[0]), [-1, L]])

                        def sb_s1(cx, i):
                            L = 128 * (i + 1)
                            nch = (L + 511) // 512
                            for ch in range(nch):
                                cw = min(512, L - ch * 512)
                                bk = cx["zb"][ch % 2]
                                cs = slice(ch * 512, ch * 512 + cw)
                                mm(ps[bk][:, 0:cw], cx["qT"][:, i * 128:(i + 1) * 128], cx["kT"][:, cs], True, True, [cx["Bq"], cx["Bk"]], [PB[bk]])
                                S.op("act", lambda e: e.activation(cx["bt"][:, cs], ps[bk][:, 0:cw], AF.Sigmoid), [PB[bk]], [cx["Bbt"]])
                                S.op("act", lambda e: e.activation(cx["ob"][:, cs], ps[bk][:, 0:cw], AF.Sigmoid, scale=-1.0), [PB[bk]], [cx["Bob"]])
                            dsl = slice(L - 128, L)
                            S.op("dve", lambda e: e.tensor_tensor(cx["bt"][:, dsl], cx["bt"][:, dsl], lt_bf, ALU.mult), [cx["Bbt"], Bcm], [cx["Bbt"]])
                            S.op("dve", lambda e: e.tensor_tensor(cx["ob"][:, dsl], cx["ob"][:, dsl], cmf_lt[:], ALU.mult), [cx["Bob"], Bcm], [cx["Bob"]])
                            S.op("dve", lambda e: e.tensor_tensor(cx["ob"][:, dsl], cx["ob"][:, dsl], notlt[:], ALU.add), [cx["Bob"], Bcm], [cx["Bob"]])

                        def sb_s2(cx, i):
                            L = 128 * (i + 1)
                            S.op("dve", lambda e: e.tensor_tensor_scan(rev(cx["R"], L), rev(cx["ob"], L), rev(cx["ob"], L), 1.0, ALU.mult, ALU.bypass),
                                 [cx["Bob"]], [cx["BR"]])
                            S.op("dve", lambda e: e.tensor_tensor(cx["w"][:, 0:L - 1], cx["bt"][:, 0:L - 1], cx["R"][:, 1:L], ALU.mult),
                                 [cx["Bbt"], cx["BR"]], [cx["Bw"]])
                            S.op("dve", lambda e: e.tensor_copy(cx["w"][:, L - 1:L], cx["bt"][:, L - 1:L]), [cx["Bbt"]], [cx["Bw"]])

                        def sb_s4(cx, i):
                            kb = 0
                            tb = cx["tb"]
                            while kb <= i:
                                n4 = min(4, i + 1 - kb)
                                for j in range(n4):
                                    S.op("pe", lambda e: e.matmul(ps[tb][:, j * 128:(j + 1) * 128], cx["w"][:, (kb + j) * 128:(kb + j + 1) * 128], ident_bf,
                                                                  start=True, stop=True), [cx["Bw"], Bcm], [PB[tb]], inc=(j == n4 - 1))
                                S.op("act", lambda e: e.activation(cx["wT"][:, kb * 128:(kb + n4) * 128], ps[tb][:, 0:n4 * 128], AF.Copy), [PB[tb]], [cx["BwT"]])
                                kb += n4

                        def sb_s5(cx, i, h):
                            ok = cx["obk"]
                            for kb in range(i + 1):
                                mm(ps[ok][:, 0:128], vall[:, kb, h * 128:(h + 1) * 128], cx["wT"][:, kb * 128:(kb + 1) * 128], kb == 0, kb == i,
                                   [Bv, cx["BwT"]], [PB[ok]])
                            S.op("dve", lambda e: e.tensor_copy(cx["oT"][:, i * 128:(i + 1) * 128], ps[ok][:, 0:128]), [PB[ok]], [cx["Bo"]])

                        for pair in range(2 if "A_v" not in stages else 0):
                            for c in range(2):
                                h = 2 * pair + c
                                cx = ctxs[c]
                                t, b, ncj = wjob("sbqk%d" % h)

                                def epi_q(tg, bk, cx=cx):
                                    evac(cx["qT"][:, tg * 512:(tg + 1) * 512], ps[bk][:, :], [PB[bk]], [cx["Bq"]], scale=128.0 ** -0.5)

                                def epi_k(tg, bk, cx=cx):
                                    evac(cx["kT"][:, tg * 512:(tg + 1) * 512], ps[bk][:, :], [PB[bk]], [cx["Bk"]])
                                lin_fm(t, b, 0, 128, KC, h_rhs, [Bh], epi_q, ncj)
                                lin_fm(t, b, 128, 128, KC, h_rhs, [Bh], epi_k, ncj)
                            items = [(c, i) for i in range(NT if "att1" not in stages else 1) for c in range(2)]
                            nit = len(items)
                            for k in range(nit + 3):
                                if k < nit:
                                    sb_s1(ctxs[items[k][0]], items[k][1])
                                if 0 <= k - 1 < nit:
                                    sb_s2(ctxs[items[k - 1][0]], items[k - 1][1])
                                if 0 <= k - 2 < nit:
                                    sb_s4(ctxs[items[k - 2][0]], items[k - 2][1])
                                if 0 <= k - 3 < nit:
                                    sb_s5(ctxs[items[k - 3][0]], items[k - 3][1], 2 * pair + items[k - 3][0])
                            for c in range(2):
                                h = 2 * pair + c
                                S.dma("sp", ODS[c], obT[0 * 512 + h * 128:0 * 512 + (h + 1) * 128, :], ctxs[c]["oT"][:], reads=[ctxs[c]["Bo"]])
                        S.barrier()


                if "B" in stages:
                    with ExitStack() as bs:
                        vh = sb("b_vh", [128, NT, 128], BF16, bs)
                        Bvh = Buf("b_vh")
                        cn = sb("b_cn", [128, 4, T], BF16, bs)
                        Bcn = Buf("b_cn")
                        kpe = sb("b_kpe", [128, T], BF16, bs)
                        Bkpe = Buf("b_kpe")
                        qk = [(sb("b_q%d" % i, [128, T], BF16, bs), Buf("b_q%d" % i), sb("b_k%d" % i, [128, T], BF16, bs), Buf("b_k%d" % i)) for i in range(1)] * 2
                        qpes = [(sb("b_qpe%d" % i, [128, T], BF16, bs), Buf("b_qpe%d" % i)) for i in range(1)] * 2
                        oTb = [(sb("b_o%d" % i, [128, T], BF16, bs), Buf("b_o%d" % i)) for i in range(1)] * 2
                        cosM = sb("b_cos", [128, T], BF16, bs)
                        sinM = sb("b_sin", [128, T], BF16, bs)
                        S.dma("sp", misc_ds, cosM[:], tabD[0], writes=[Btab])
                        S.dma("sp", misc_ds, sinM[:], tabD[1], writes=[Btab])
                        mctx = []
                        for c in range(2):
                            mctx.append(dict(
                                zc=sb("b_zc%d" % c, [128, T], F32, bs), Bzc=Buf("b_zc%d" % c),
                                w=sb("b_w%d" % c, [128, T], BF16, bs), Bw=Buf("b_w%d" % c),
                                wT=sb("b_wT%d" % c, [128, T], BF16, bs), BwT=Buf("b_wT%d" % c),
                                col=sb("b_col%d" % c, [128, 4], F32, bs), Bcol=Buf("b_col%d" % c),
                                dg=sb("b_dg%d" % c, [128, 128], BF16, bs), Bdg=Buf("b_dg%d" % c),
                                zb=(0, 1) if c == 0 else (4, 5), tb=2 if c == 0 else 6, obk=3 if c == 0 else 7))
                        cf = sb("b_cf", [128, 4, 512], BF16, bs)
                        Bcf = Buf("b_cf")
                        sq = [sb("b_sq%d" % i, [128, 512], BF16, bs) for i in range(2)]
                        Bsq = [Buf("b_sq%d" % i) for i in range(2)]
                        rsb = sb("b_rs", [128, 512], F32, bs)
                        Brs = Buf("b_rs")
                        rt = (sb("b_xs", [128, 512], BF16, bs), sb("b_t1", [128, 512], F32, bs), sb("b_t2", [128, 512], F32, bs),
                              Buf("b_xs"), Buf("b_t1"), Buf("b_t2"))
                        jobs = [wjob("ckv0"), wjob("ckv1")]
                        lin_nb[0] = 3
                        for tg in range(4):
                            bsum = 3
                            for c in range(4):
                                t, b, ncj = jobs[c // 2]

                                def epi_c(tg_, bk, c=c):
                                    S.op("act", lambda e: e.activation(sq[c % 2][:], ps[bk][:, :], AF.Square), [PB[bk]], [Bsq[c % 2]])
                                    S.op("dve", lambda e: e.tensor_copy(cf[:, c, :], ps[bk][:, :]), [PB[bk]], [Bcf])
                                lin_fm(t, b, (c % 2) * 128, 128, KC, h_rhs, [Bh], epi_c, ncj, tgs=[tg])
                                mm(ps[bsum][:, :], ones_bf[:, 0:128], sq[c % 2][:], c == 0, c == 3, [Bones, Bsq[c % 2]], [PB[bsum]], inc=True)
                            S.op("act", lambda e: e.activation(rsb[:], ps[bsum][:, :], AF.Sqrt, bias=ccs[:, CC_EPS:CC_EPS + 1], scale=1.0 / 512.0),
                                 [PB[bsum], Bcc], [Brs])
                            S.op("dve", lambda e: e.reciprocal(rsb[:], rsb[:]), [Brs], [Brs])
                            for c in range(4):
                                S.op("dve", lambda e: e.scalar_tensor_tensor(cn[:, c, tg * 512:(tg + 1) * 512], cf[:, c, :],
                                                                              pvs[:, PV_KVG + c:PV_KVG + c + 1], rsb[:], ALU.mult, ALU.mult),
                                     [Bcf, Brs, Bpv], [Bcn])
                        lin_nb[0] = 4
                        t, b, ncj = wjob("kr")

                        def epi_kr(tg, bk):
                            rope_fm(bk, 64, cosM, sinM, r64_bf, 1.0, kpe[0:64, tg * 512:(tg + 1) * 512], Bkpe, tg, rt)
                        lin_fm(t, b, 0, 64, KC, h_rhs, [Bh], epi_kr, ncj)
                        def cn_rhs(kc, tg):
                            return cn[:, kc, tg * 512:(tg + 1) * 512]

                        def cn_lhs(kc, tile):
                            return cn[:, kc, tile * 128:(tile + 1) * 128]
                        sc = 192.0 ** -0.5

                        def m_s1(cx, i, qT, Bq, kT, Bk, qpe, Bqpe):
                            L = 128 * (i + 1)
                            nch = (L + 511) // 512
                            for ch in range(nch):
                                cw = min(512, L - ch * 512)
                                bk = cx["zb"][ch % 2]
                                cs = slice(ch * 512, ch * 512 + cw)
                                mm(ps[bk][:, 0:cw], qT[:, i * 128:(i + 1) * 128], kT[:, cs], True, False, [Bq, Bk], [PB[bk]])
                                mm(ps[bk][:, 0:cw], qpe[0:64, i * 128:(i + 1) * 128], kpe[0:64, cs], False, True, [Bqpe, Bkpe], [PB[bk]])
                                evac(cx["zc"][:, cs], ps[bk][:, 0:cw], [PB[bk]], [cx["Bzc"]])
                            dsl = slice(L - 128, L)
                            S.op("dve", lambda e: e.tensor_tensor(cx["zc"][:, dsl], cx["zc"][:, dsl], cmf[:, 0, :], ALU.add), [cx["Bzc"], Bcm], [cx["Bzc"]])

                        def m_s2(cx, i):
                            L = 128 * (i + 1)
                            col, Bcol = cx["col"], cx["Bcol"]
                            S.op("dve", lambda e: e.tensor_reduce(col[:, 0:1], cx["zc"][:, 0:L], AX.X, ALU.max, negate=True), [cx["Bzc"]], [Bcol])
                            S.op("act", lambda e: e.activation(cx["w"][:, 0:L], cx["zc"][:, 0:L], AF.Exp, bias=col[:, 0:1], accum_out=col[:, 1:2]),
                                 [cx["Bzc"], Bcol], [cx["Bw"], Bcol])
                            S.op("dve", lambda e: e.reciprocal(col[:, 2:3], col[:, 1:2]), [Bcol], [Bcol])
                            S.op("dve", lambda e: e.tensor_scalar(cx["dg"][:], ident_f[:], col[:, 2:3], None, ALU.mult), [Bcm, Bcol], [cx["Bdg"]])

                        def m_s3(cx, i):
                            kb = 0
                            tb = cx["tb"]
                            while kb <= i:
                                n4 = min(4, i + 1 - kb)
                                for j in range(n4):
                                    S.op("pe", lambda e: e.matmul(ps[tb][:, j * 128:(j + 1) * 128], cx["w"][:, (kb + j) * 128:(kb + j + 1) * 128], cx["dg"][:],
                                                                  start=True, stop=True), [cx["Bw"], cx["Bdg"]], [PB[tb]], inc=(j == n4 - 1))
                                evac(cx["wT"][:, kb * 128:(kb + n4) * 128], ps[tb][:, 0:n4 * 128], [PB[tb]], [cx["BwT"]])
                                kb += n4

                        def m_s4(cx, i, oT, Bo):
                            ok = cx["obk"]
                            for kb in range(i + 1):
                                mm(ps[ok][:, 0:128], vh[:, kb, :], cx["wT"][:, kb * 128:(kb + 1) * 128], kb == 0, kb == i, [Bvh, cx["BwT"]], [PB[ok]])
                            evac(oT[:, i * 128:(i + 1) * 128], ps[ok][:, 0:128], [PB[ok]], [Bo])

                        for h in range(4):
                            qT, Bq, kT, Bk = qk[h % 2]
                            qpe, Bqpe = qpes[h % 2]
                            oT, Bo = oTb[h % 2]
                            t, b, ncj = wjob("mq%d" % h)

                            def epi_q(tg, bk, qT=qT, Bq=Bq):
                                evac(qT[:, tg * 512:(tg + 1) * 512], ps[bk][:, :], [PB[bk]], [Bq], scale=sc)

                            def epi_qpe(tg, bk, qpe=qpe, Bqpe=Bqpe):
                                rope_fm(bk, 64, cosM, sinM, r64_bf, sc, qpe[0:64, tg * 512:(tg + 1) * 512], Bqpe, tg, rt)
                            lin_fm(t, b, 0, 128, KC, h_rhs, [Bh], epi_q, ncj)
                            lin_fm(t, b, 128, 64, KC, h_rhs, [Bh], epi_qpe, ncj)
                            t, b = wload(ukv[l, :, h * 512:(h + 1) * 512], 512)

                            def epi_k(tg, bk, kT=kT, Bk=Bk):
                                evac(kT[:, tg * 512:(tg + 1) * 512], ps[bk][:, :], [PB[bk]], [Bk])
                            lin_fm(t, b, 0, 128, 4, cn_rhs, [Bcn], epi_k, 128)
                            half = h // 2
                            t, b = wload(ukv[l, :, 2048 + half * 1024:2048 + (half + 1) * 1024], 1024)

                            def epi_v(tile, bk):
                                evac(vh[:, tile, :], ps[bk][:, 0:128], [PB[bk]], [Bvh])
                            lin_tm(t, b, (h % 2) * 128, 128, 4, cn_lhs, [Bcn], epi_v, 256)
                            for k in range(NT + 3):
                                if k < NT:
                                    m_s1(mctx[k % 2], k, qT, Bq, kT, Bk, qpe, Bqpe)
                                if 0 <= k - 1 < NT:
                                    m_s2(mctx[(k - 1) % 2], k - 1)
                                if 0 <= k - 2 < NT:
                                    m_s3(mctx[(k - 2) % 2], k - 2)
                                if 0 <= k - 3 < NT:
                                    m_s4(mctx[(k - 3) % 2], k - 3, oT, Bo)
                            S.dma("sp", ODS[h % 2], obT[1 * 512 + h * 128:1 * 512 + (h + 1) * 128, :], oT[:], reads=[Bo])
                        gate_flush()
                        S.barrier()

                if "C" in stages:
                    with ExitStack() as bs:
                        vall = sb("c_v", [128, NT, 512], BF16, bs)
                        Bv = Buf("c_v")
                        sg = sb("c_sg", [128, NT, 512], BF16, bs)
                        Bsg = Buf("c_sg")
                        rt = (sb("c_xs", [128, 512], BF16, bs), sb("c_t1", [128, 512], F32, bs), sb("c_t2", [128, 512], F32, bs),
                              Buf("c_xs"), Buf("c_t1"), Buf("c_t2"))
                        cctx = []
                        for c in range(2):
                            cx = dict(
                                qr=sb("c_qr%d" % c, [128, T], BF16, bs), Bqr=Buf("c_qr%d" % c),
                                kr=sb("c_kr%d" % c, [128, T], BF16, bs), Bkr=Buf("c_kr%d" % c),
                                qd=sb("c_qd%d" % c, [128, T], BF16, bs), Bqd=Buf("c_qd%d" % c),
                                oT=sb("c_o%d" % c, [128, T], BF16, bs), Bo=Buf("c_o%d" % c),
                                stf=sb("c_stf%d" % c, [128, 128], F32, bs), Bstf=Buf("c_stf%d" % c),
                                stb=[(sb("c_stb%d_%d" % (c, i), [128, 128], BF16, bs), Buf("c_stb%d_%d" % (c, i))) for i in range(2)],
                                sTm=[(sb("c_sTm%d_%d" % (c, i), [128, 128], BF16, bs), Buf("c_sTm%d_%d" % (c, i))) for i in range(2)],
                                kd=[(sb("c_kd%d_%d" % (c, i), [128, 128], BF16, bs), Buf("c_kd%d_%d" % (c, i))) for i in range(2)],
                                og=[(sb("c_og%d_%d" % (c, i), [128, 128], BF16, bs), Buf("c_og%d_%d" % (c, i))) for i in range(2)],
                                junk=sb("c_junk%d" % c, [128, 128], F32, bs), Bjunk=Buf("c_junk%d" % c),
                                col=sb("c_col%d" % c, [128, 4], F32, bs), Bcol=Buf("c_col%d" % c),
                                pb=(0, 1, 2, 3) if c == 0 else (4, 5, 6, 7))
                            cctx.append(cx)
                        cosR = sb("c_cos", [128, T], BF16, bs)
                        sinR = sb("c_sin", [128, T], BF16, bs)
                        S.dma("sp", misc_ds, cosR[:], tabD[2], writes=[Btab])
                        S.dma("sp", misc_ds, sinR[:], tabD[3], writes=[Btab])
                        for half in range(2):
                            t, b, ncj = wjob("rv%d" % half)

                            def epi_v(tile, bk, half=half):
                                evac(vall[:, tile, half * 256:(half + 1) * 256], ps[bk][:, 0:256], [PB[bk]], [Bv])
                            lin_tm(t, b, 0, 256, KC, h_lhs, [Bh], epi_v, ncj)
                        for half in range(2):
                            t, b, ncj = wjob("rg%d" % half)

                            def epi_g(tile, bk, half=half):
                                S.op("act", lambda e: e.activation(sg[:, tile, half * 256:(half + 1) * 256], ps[bk][:, 0:256], AF.Silu), [PB[bk]], [Bsg])
                            lin_tm(t, b, 0, 256, KC, h_lhs, [Bh], epi_g, ncj)

                        def c_s1(cx, h, n):
                            csl = slice(n * 128, (n + 1) * 128)
                            sT_, BsT = cx["sTm"][n % 2]
                            b0 = cx["pb"][0]
                            mm(ps[b0][:, 0:128], cx["kr"][:, csl], cx["qr"][:, csl], True, True, [cx["Bkr"], cx["Bqr"]], [PB[b0]])
                            S.op("dve", lambda e: e.tensor_tensor(sT_[:], ps[b0][:, 0:128], cmf[:, 1 + h, :], ALU.mult), [PB[b0], Bcm], [BsT])

                        def c_s2(cx, h, n):
                            csl = slice(n * 128, (n + 1) * 128)
                            hs = slice(h * 128, (h + 1) * 128)
                            sT_, BsT = cx["sTm"][n % 2]
                            og_, Bog = cx["og"][n % 2]
                            b1 = cx["pb"][1]
                            col, Bcol = cx["col"], cx["Bcol"]
                            mm(ps[b1][:, 0:128], sT_[:], vall[:, n, hs], True, n == 0, [BsT, Bv], [PB[b1]])
                            if n > 0:
                                sb_, Bsb = cx["stb"][n % 2]
                                mm(ps[b1][:, 0:128], cx["qd"][:, csl], sb_[:], False, True, [cx["Bqd"], Bsb], [PB[b1]])
                            S.op("act", lambda e: e.activation(cx["junk"][:], ps[b1][:, 0:128], AF.Square, accum_out=col[:, 0:1]), [PB[b1]], [cx["Bjunk"], Bcol])
                            S.op("act", lambda e: e.activation(col[:, 1:2], col[:, 0:1], AF.Sqrt, bias=ccs[:, CC_EPS:CC_EPS + 1], scale=1.0 / 128.0),
                                 [Bcol, Bcc], [Bcol])
                            S.op("dve", lambda e: e.reciprocal(col[:, 2:3], col[:, 1:2]), [Bcol], [Bcol])
                            S.op("dve", lambda e: e.scalar_tensor_tensor(og_[:], ps[b1][:, 0:128], col[:, 2:3], sg[:, n, hs], ALU.mult, ALU.mult),
                                 [PB[b1], Bcol, Bsg], [Bog])

                        def c_s3(cx, h, n):
                            csl = slice(n * 128, (n + 1) * 128)
                            og_, Bog = cx["og"][n % 2]
                            b2 = cx["pb"][2]
                            mm(ps[b2][:, 0:128], og_[:], ident_bf, True, True, [Bog, Bcm], [PB[b2]])
                            evac(cx["oT"][:, csl], ps[b2][:, 0:128], [PB[b2]], [cx["Bo"]])

                        def c_s4(cx, h, n):
                            if n >= NT - 1:
                                return
                            csl = slice(n * 128, (n + 1) * 128)
                            hs = slice(h * 128, (h + 1) * 128)
                            kd_, Bkd = cx["kd"][n % 2]
                            b3 = cx["pb"][3]
                            stf, Bstf = cx["stf"], cx["Bstf"]
                            mm(ps[b3][:, 0:128], cx["kr"][:, csl], ident_bf, True, True, [cx["Bkr"], Bcm], [PB[b3]])
                            S.op("act", lambda e: e.activation(kd_[:], ps[b3][:, 0:128], AF.Copy, scale=ccs[:, CC_KDEC + h:CC_KDEC + h + 1]),
                                 [PB[b3], Bcc], [Bkd])
                            mm(ps[b3][:, 128:256], kd_[:], vall[:, n, hs], True, True, [Bkd, Bv], [PB[b3]])
                            if n == 0:
                                S.op("dve", lambda e: e.tensor_copy(stf[:], ps[b3][:, 128:256]), [PB[b3]], [Bstf])
                            else:
                                S.op("dve", lambda e: e.scalar_tensor_tensor(stf[:], stf[:], CD[h], ps[b3][:, 128:256], ALU.mult, ALU.add),
                                     [Bstf, PB[b3]], [Bstf])
                            sbn, Bsbn = cx["stb"][(n + 1) % 2]
                            S.op("act", lambda e: e.activation(sbn[:], stf[:], AF.Copy), [Bstf], [Bsbn])

                        for pair in range(2):
                            for c in range(2):
                                h = 2 * pair + c
                                cx = cctx[c]
                                t, b, ncj = wjob("rqk%d" % h)

                                def epi_q(tg, bk, cx=cx):
                                    rope_fm(bk, 128, cosR, sinR, r128_bf, 1.0, cx["qr"][:, tg * 512:(tg + 1) * 512], cx["Bqr"], tg, rt)

                                def epi_k(tg, bk, cx=cx):
                                    rope_fm(bk, 128, cosR, sinR, r128_bf, 128.0 ** -0.5, cx["kr"][:, tg * 512:(tg + 1) * 512], cx["Bkr"], tg, rt)
                                lin_fm(t, b, 0, 128, KC, h_rhs, [Bh], epi_q, ncj)
                                lin_fm(t, b, 128, 128, KC, h_rhs, [Bh], epi_k, ncj)
                                for n in range(NT):
                                    csl = slice(n * 128, (n + 1) * 128)
                                    S.op("dve", lambda e: e.tensor_tensor(cx["qd"][:, csl], cx["qr"][:, csl], cmf[:, 5 + h, :], ALU.mult), [cx["Bqr"], Bcm], [cx["Bqd"]])
                            for n in range(NT):
                                for stg in (c_s1, c_s2, c_s3, c_s4):
                                    for c in range(2):
                                        stg(cctx[c], 2 * pair + c, n)
                            for c in range(2):
                                h = 2 * pair + c
                                S.dma("sp", ODS[c], obT[2 * 512 + h * 128:2 * 512 + (h + 1) * 128, :], cctx[c]["oT"][:], reads=[cctx[c]["Bo"]])
                        gate_flush()
                        S.barrier()


                if "D" in stages:
                    with ExitStack() as bs:
                        names = ["xf", "yf", "xc", "r", "ig", "a", "bb", "hh"]
                        Lb = {n: sb("d_" + n, [128, T], F32, bs) for n in names}
                        LB = {n: Buf("d_" + n) for n in names}
                        xcb = sb("d_xcb", [128, T], BF16, bs)
                        Bxcb = Buf("d_xcb")
                        oT = sb("d_o", [128, T], BF16, bs)
                        Bo = Buf("d_o")
                        waxs = sb("d_wax", [128, 1024], BF16, bs)
                        Bwax = Buf("d_wax")
                        nsp = sb("d_nsp", [128, 8], F32, bs)
                        Bnsp = Buf("d_nsp")
                        S.barrier(only=("pool",))
                        S.dma("pool", misc_ds, waxs[:], wax[l], writes=[Bwax])
                        lin_nb[0] = 3
                        gbanks[0] = [3, 4, 5, 6, 7]
                        hk = [0]

                        def d_hook():
                            hk[0] += 1
                            tick(1 + (hk[0] % 2))
                        S.hook = d_hook
                        S.op("act", lambda e: e.activation(nsp[:, 0:4], pvs[:, PV_LAM:PV_LAM + 4], AF.Exp, scale=-1.0), [Bpv], [Bnsp])
                        S.op("act", lambda e: e.activation(nsp[:, 0:4], nsp[:, 0:4], AF.Ln, bias=ccs[:, CC_ONE:CC_ONE + 1]), [Bnsp, Bcc], [Bnsp])
                        S.op("dve", lambda e: e.tensor_scalar(nsp[:, 4:8], nsp[:, 0:4], -16.0, None, ALU.mult), [Bnsp], [Bnsp])
                        S.op("dve", lambda e: e.tensor_scalar(nsp[:, 0:4], nsp[:, 0:4], -8.0, None, ALU.mult), [Bnsp], [Bnsp])
                        for c in range(4):
                            xf, yf, xc, r_, ig, a_, bb, hh = [Lb[n] for n in names]
                            t, b, ncj = wjob("lxy%d" % c)

                            def epi_x(tg, bk):
                                evac(xf[:, tg * 512:(tg + 1) * 512], ps[bk][:, :], [PB[bk]], [LB["xf"]])

                            def epi_y(tg, bk):
                                evac(yf[:, tg * 512:(tg + 1) * 512], ps[bk][:, :], [PB[bk]], [LB["yf"]])
                            lin_fm(t, b, 0, 128, KC, h_rhs, [Bh], epi_x, ncj)
                            lin_fm(t, b, 128, 128, KC, h_rhs, [Bh], epi_y, ncj)

                            def cwc(tap):
                                return pvs[:, PV_CW + 4 * tap + c:PV_CW + 4 * tap + c + 1]
                            S.op("dve", lambda e: e.tensor_scalar(xc[:], xf[:], cwc(3), pvs[:, PV_CB + c:PV_CB + c + 1], ALU.mult, ALU.add),
                                 [LB["xf"], Bpv], [LB["xc"]])
                            for tap, sh in ((2, 1), (1, 2), (0, 3)):
                                S.op("dve", lambda e: e.scalar_tensor_tensor(xc[:, sh:T], xf[:, 0:T - sh], cwc(tap), xc[:, sh:T], ALU.mult, ALU.add),
                                     [LB["xf"], LB["xc"], Bpv], [LB["xc"]])
                            S.op("act", lambda e: e.activation(xcb[:], xc[:], AF.Copy), [LB["xc"]], [Bxcb])
                            for tg in range(4):
                                sl = slice(tg * 512, (tg + 1) * 512)
                                bk = lin_bank()
                                mm(ps[bk][:, :], waxs[:, c * 128:(c + 1) * 128], xcb[:, sl], True, True, [Bwax, Bxcb], [PB[bk]])
                                S.op("act", lambda e: e.activation(r_[:, sl], ps[bk][:, :], AF.Sigmoid, bias=pvs[:, PV_BA + c:PV_BA + c + 1]),
                                     [PB[bk], Bpv], [LB["r"]])
                                bk = lin_bank()
                                mm(ps[bk][:, :], waxs[:, 512 + c * 128:512 + (c + 1) * 128], xcb[:, sl], True, True, [Bwax, Bxcb], [PB[bk]])
                                S.op("act", lambda e: e.activation(ig[:, sl], ps[bk][:, :], AF.Sigmoid, bias=pvs[:, PV_BX + c:PV_BX + c + 1]),
                                     [PB[bk], Bpv], [LB["ig"]])
                            S.op("act", lambda e: e.activation(a_[:], r_[:], AF.Exp, scale=nsp[:, c:c + 1]), [LB["r"], Bnsp], [LB["a"]])
                            S.op("act", lambda e: e.activation(bb[:], r_[:], AF.Exp, scale=nsp[:, 4 + c:5 + c]), [LB["r"], Bnsp], [LB["bb"]])
                            S.op("dve", lambda e: e.tensor_scalar(bb[:], bb[:], -1.0, 1.0, ALU.mult, ALU.add), [LB["bb"]], [LB["bb"]])
                            S.op("dve", lambda e: e.tensor_scalar(bb[:], bb[:], 0.0, None, ALU.max), [LB["bb"]], [LB["bb"]])
                            S.op("act", lambda e: e.activation(bb[:], bb[:], AF.Sqrt), [LB["bb"]], [LB["bb"]])
                            S.op("dve", lambda e: e.tensor_tensor(ig[:], ig[:], xc[:], ALU.mult), [LB["ig"], LB["xc"]], [LB["ig"]])
                            S.op("dve", lambda e: e.tensor_tensor(bb[:], bb[:], ig[:], ALU.mult), [LB["bb"], LB["ig"]], [LB["bb"]])
                            S.op("dve", lambda e: e.tensor_tensor_scan(hh[:], a_[:], bb[:], 0.0, ALU.mult, ALU.add), [LB["a"], LB["bb"]], [LB["hh"]])
                            S.op("dve", lambda e: e.tensor_tensor(r_[:], yf[:], yf[:], ALU.mult), [LB["yf"]], [LB["r"]])
                            S.op("dve", lambda e: e.tensor_scalar(r_[:], r_[:], 0.044715, 1.0, ALU.mult, ALU.add), [LB["r"]], [LB["r"]])
                            S.op("dve", lambda e: e.tensor_tensor(r_[:], r_[:], yf[:], ALU.mult), [LB["r"], LB["yf"]], [LB["r"]])
                            S.op("act", lambda e: e.activation(r_[:], r_[:], AF.Sigmoid, scale=2.0 * math.sqrt(2.0 / math.pi)), [LB["r"]], [LB["r"]])
                            S.op("dve", lambda e: e.tensor_tensor(r_[:], r_[:], yf[:], ALU.mult), [LB["r"], LB["yf"]], [LB["r"]])
                            S.op("dve", lambda e: e.tensor_tensor(oT[:], hh[:], r_[:], ALU.mult), [LB["hh"], LB["r"]], [Bo])
                            S.dma("sp", ODS[c % 2], obT[3 * 512 + c * 128:3 * 512 + (c + 1) * 128, :], oT[:], reads=[Bo])
                        S.hook = None
                        gate_flush()
                        lin_nb[0] = 4
                        S.barrier()

                if "gate" in stages:
                    gate_flush()
                    gbanks[0] = [0, 1, 2, 3]
                    for _ in ggen:
                        pass
                    gate_flush()
                    lin_nb[0] = 4
                    S.barrier()
                    with ExitStack() as bs:
                        ob = sb("g_ob", [128, 16, T], BF16, bs)
                        Bob = Buf("g_ob")
                        acc = sb("g_acc", [128, T], F32, bs)
                        Bacc = Buf("g_acc")
                        tmp4 = [(sb("g_tmp%d" % i, [128, T], F32, bs), Buf("g_tmp%d" % i)) for i in range(1)] * 2
                        mixc = sb("g_mix", [128, T], BF16, bs)
                        Bmix = Buf("g_mix")
                        wbs = [(sb("g_wb%d" % i, [128, 2048], BF16, bs), Buf("g_wb%d" % i)) for i in range(2)]
                        sgm = [(hbuf[:, 4 * i:4 * i + 4, :], Buf("g_sgm%d" % i)) for i in range(2)]
                        obv = obT.rearrange("(c p) t -> p c t", p=128)
                        for q4 in range(4):
                            S.dma("sp", misc_ds, ob[:, q4 * 4:(q4 + 1) * 4, :], obv[:, q4 * 4:(q4 + 1) * 4, :], writes=[Bob])
                        ti = 0
                        S.barrier(only=("pool",))
                        def load_sg(mm_):
                            S.dma("sp", XDS[mm_ % 2], sgm[mm_ % 2][0], sgD[:, mm_ * 128:(mm_ + 1) * 128, :].rearrange("b p t -> p b t"),
                                  writes=[sgm[mm_ % 2][1]])
                        load_sg(0)
                        for m in range(16):
                            sg_t, sg_b = sgm[m % 2]
                            if m + 1 < 16:
                                load_sg(m + 1)
                            wb_t, wb_b = wbs[m % 2]
                            S.dma("pool", WBDS[m % 2], wb_t[:], wbr[l, m], writes=[wb_b])
                            tbv = wb_t[:, :].rearrange("p (b c j) -> p b c j", b=4, c=4)
                            for b in range(4):
                                tmp, Btmp = tmp4[ti % 2]
                                ti += 1
                                for tg in range(4):
                                    sl = slice(tg * 512, (tg + 1) * 512)
                                    bkp = lin_bank()
                                    for kc in range(4):
                                        mm(ps[bkp][:, :], tbv[:, b, kc, :], ob[:, b * 4 + kc, sl], kc == 0, kc == 3, [wb_b, Bob], [PB[bkp]])
                                    if b == 0:
                                        S.op("dve", lambda e: e.tensor_tensor(acc[:, sl], sg_t[:, b, sl], ps[bkp][:, :], ALU.mult), [sg_b, PB[bkp]], [Bacc])
                                    else:
                                        S.op("dve", lambda e: e.tensor_tensor(tmp[:, sl], sg_t[:, b, sl], ps[bkp][:, :], ALU.mult), [sg_b, PB[bkp]], [Btmp])
                                if 0 < b < 3:
                                    S.op("dve", lambda e: e.tensor_tensor(acc[:], acc[:], tmp[:], ALU.add), [Bacc, Btmp], [Bacc])
                                elif b == 3:
                                    S.op("dve", lambda e: e.tensor_tensor(mixc[:], acc[:], tmp[:], ALU.add), [Bacc, Btmp], [Bmix])
                            S.dma("sp", ODS[m % 2], mixT[m * 128:(m + 1) * 128, :], mixc[:], reads=[Bmix])
                        S.barrier()
                    wl_n[0] = 3


                if "wout" in stages:
                    with ExitStack() as bs:
                        xt = [(sb("o_x%d" % i, [128, T], F32, bs), Buf("o_x%d" % i)) for i in range(2)]
                        xds = XDS
                        mv = mixT.rearrange("(c p) t -> p c t", p=128)
                        for q4 in range(4):
                            S.dma("sp", misc_ds, hbuf[:, q4 * 4:(q4 + 1) * 4, :], mv[:, q4 * 4:(q4 + 1) * 4, :], writes=[Bh])
                        def load_x(mm_):
                            S.dma("sp", xds[mm_ % 2], xt[mm_ % 2][0][:], (xT_in if l == 0 else xres)[mm_ * 128:(mm_ + 1) * 128, :], writes=[xt[mm_ % 2][1]])
                        load_x(0)
                        for m in range(16):
                            x_, Bx_ = xt[m % 2]
                            if m + 1 < 16:
                                load_x(m + 1)
                            t, b = wload(wout[l, m], 2048)
                            wv = t[:, 0:2048].rearrange("p (c j) -> p c j", j=128)
                            for tg in range(4):
                                sl = slice(tg * 512, (tg + 1) * 512)
                                bk = lin_bank()
                                for kc in range(KC):
                                    mm(ps[bk][:, :], wv[:, kc, :], hbuf[:, kc, sl], kc == 0, kc == KC - 1, [b, Bh], [PB[bk]])
                                S.op("dve", lambda e: e.tensor_tensor(x_[:, sl], x_[:, sl], ps[bk][:, :], ALU.add), [Bx_, PB[bk]], [Bx_])
                            S.dma("sp", xds[m % 2], xres[m * 128:(m + 1) * 128, :], x_[:], reads=[Bx_])
                        S.barrier()

            if "ffn" in stages:
                norm_phase(xres, pvs, PV_LN2, False)
                with ExitStack() as bs:
                    aT = sb("f_a", [128, FC, 512], BF16, bs)
                    Ba = Buf("f_a")
                    sgf = [(sb("f_sg%d" % i, [128, 512], F32, bs), Buf("f_sg%d" % i)) for i in range(2)]
                    xq = [(sb("f_x%d" % i, [128, 512], F32, bs), Buf("f_x%d" % i)) for i in range(2)]
                    fds = FDS
                    for tq in range(4):
                        sl = slice(tq * 512, (tq + 1) * 512)
                        for f in range(FC):
                            t, b = wload(wfgu[l, f], 4096)
                            wv = t[:, 0:4096].rearrange("p (g c j) -> p g c j", g=2, c=KC)
                            bkg = lin_bank()
                            for kc in range(KC):
                                mm(ps[bkg][:, :], wv[:, 0, kc, :], hbuf[:, kc, sl], kc == 0, kc == KC - 1, [b, Bh], [PB[bkg]])
                            bku = lin_bank()
                            for kc in range(KC):
                                mm(ps[bku][:, :], wv[:, 1, kc, :], hbuf[:, kc, sl], kc == 0, kc == KC - 1, [b, Bh], [PB[bku]])
                            sg_, Bsg_ = sgf[f % 2]
                            S.op("act", lambda e: e.activation(sg_[:], ps[bkg][:, :], AF.Silu), [PB[bkg]], [Bsg_])
                            S.op("dve", lambda e: e.tensor_tensor(aT[:, f, :], sg_[:], ps[bku][:, :], ALU.mult), [Bsg_, PB[bku]], [Ba])
                        for m in range(16):
                            x_, Bx_ = xq[m % 2]
                            S.dma("sp", fds[m % 2], x_[:], xres[m * 128:(m + 1) * 128, sl], writes=[Bx_])
                            t, b = wload(wfd[l, m], FC * 128)
                            wv = t[:, 0:FC * 128].rearrange("p (c j) -> p c j", j=128)
                            bk = lin_bank()
                            for f in range(FC):
                                mm(ps[bk][:, :], wv[:, f, :], aT[:, f, :], f == 0, f == FC - 1, [b, Ba], [PB[bk]])
                            S.op("dve", lambda e: e.tensor_tensor(x_[:], x_[:], ps[bk][:, :], ALU.add), [Bx_, PB[bk]], [Bx_])
                            S.dma("sp", fds[m % 2], xres[m * 128:(m + 1) * 128, sl], x_[:], reads=[Bx_])
                    S.barrier()

        if final_norm:
            norm_phase(xres, lnfs, 0, True)
        S.barrier(skip=())
        for ds in S.dsems:
            if ds.cnt > 0:
                nc.sync.wait_ge(ds.sem, ds.cnt)
        build._ninst = S.ninst
        build._stuck = S.check_deadlock()
    return nc


def _common_inputs(inp):
    cc, cm, _ = _consts()
    return {"cc": cc, "cm": cm, "lnf": _col128(np.asarray(inp["ln_final"], np.float32))}


def kernel(**inputs):
    inp = {k: np.asarray(v) for k, v in inputs.items()}
    n = 8
    nc = build(DEPTH, final_norm=True)
    W = prep_weights(inp, list(range(DEPTH)))
    com = _common_inputs(inp)
    in_maps = []
    for c in range(n):
        m = {"xT": np.ascontiguousarray(inp["x"][c].T),
             "pos": np.ascontiguousarray(inp["positions"][c:c + 1].astype(np.int32))}
        m.update(com)
        m.update(W)
        in_maps.append(m)
    res = run_bass_kernel_spmd(nc, in_maps, core_ids=list(range(n)))
    out = np.stack([np.ascontiguousarray(res.results[c]["outT"].T) for c in range(n)], axis=0)
    return out.astype(np.float32)
```

```python
import math
from contextlib import ExitStack

import numpy as np
import concourse.bass as bass
import concourse.mybir as mybir
from concourse.bass_utils import run_bass_kernel_spmd

F32 = mybir.dt.float32
BF16 = mybir.dt.bfloat16
I32 = mybir.dt.int32
AF = mybir.ActivationFunctionType
ALU = mybir.AluOpType
AX = mybir.AxisListType

T = 2048
D = 2048
KC = 16
NT = 16
DFF = 5632
FC = 44
DEPTH = 4
EPS = 1e-6
THETA = 10000.0
TWO_PI = 2.0 * math.pi


class Dep:
    __slots__ = ("sem", "val", "eng")

    def __init__(self, sem, val, eng):
        self.sem = sem
        self.val = val
        self.eng = eng


class Buf:
    __slots__ = ("name", "w", "r")

    def __init__(self, name):
        self.name = name
        self.w = None
        self.r = {}


class DSem:
    __slots__ = ("sem", "cnt", "name")

    def __init__(self, sem, name):
        self.sem = sem
        self.cnt = 0
        self.name = name


class Eng:
    def __init__(self, name, h, is_pe=False):
        self.name = name
        self.h = h
        self.is_pe = is_pe
        self.sem = None
        self.cnt = 0
        self.pending = False
        self.known = {}


class Sy:
    EPOCH = 30000

    def __init__(self, nc, stack):
        self.nc = nc
        self.stack = stack
        self.nsem = 0
        self.E = {
            "pe": Eng("pe", nc.tensor, True),
            "dve": Eng("dve", nc.vector),
            "act": Eng("act", nc.scalar),
            "pool": Eng("pool", nc.gpsimd),
            "sp": Eng("sp", nc.sync),
        }
        for e in self.E.values():
            e.sem = self.new_sem(e.name)
        self.dsems = []
        self.ninst = 0
        self.trace = {k: [] for k in self.E}
        self.port = Buf("psum_port")
        self.hook = None
        self._inhook = False

    def new_sem(self, name):
        self.nsem += 1
        return self.stack.enter_context(self.nc.semaphore("s%d_%s" % (self.nsem, name)))

    def dsem(self, name):
        d = DSem(self.new_sem(name), name)
        self.dsems.append(d)
        return d

    def _deps(self, reads, writes):
        deps = []
        for b in reads:
            if b.w is not None:
                deps.append((b.w, "raw"))
        for b in writes:
            if b.w is not None:
                deps.append((b.w, "waw"))
            for d in b.r.values():
                deps.append((d, "war"))
        return deps

    def _wait(self, E, deps, is_dma):
        waits = {}
        for d, kind in deps:
            if (not is_dma) and d.eng == E.name:
                if kind != "raw" or E.is_pe:
                    continue
            if E.known.get(d.sem, 0) >= d.val:
                continue
            if waits.get(d.sem, (None, 0))[1] < d.val:
                waits[d.sem] = (d.sem, d.val)
        for sem, val in waits.values():
            E.h.wait_ge(sem, val)
            E.known[sem] = val
            self.ninst += 1
            self.trace[E.name].append(("w", sem.num, val))

    def op(self, en, emit, reads=(), writes=(), inc=True):
        E = self.E[en]
        if en in ("act", "dve") and any(bb.name.startswith("ps") for bb in reads):
            writes = list(writes) + [self.port]
        self._wait(E, self._deps(reads, writes), False)
        ins = emit(E.h)
        self.ninst += 1
        if inc:
            if E.cnt >= self.EPOCH and not E.pending:
                E.sem = self.new_sem(E.name)
                E.cnt = 0
            E.cnt += 1
            ins.then_inc(E.sem, 1)
            self.trace[en].append(("i", E.sem.num, 1))
            E.pending = False
            d = Dep(E.sem, E.cnt, en)
        else:
            E.pending = True
            d = Dep(E.sem, E.cnt + 1, en)
        for b in reads:
            b.r[en] = d
        for b in writes:
            b.w = d
            b.r = {}
        if self.hook is not None and not self._inhook and en in ("dve", "act"):
            self._inhook = True
            self.hook()
            self._inhook = False
        return ins

    def dma(self, qn, ds, out_ap, in_ap, reads=(), writes=()):
        E = self.E[qn]
        self._wait(E, self._deps(reads, writes), True)
        ins = E.h.dma_start(out=out_ap, in_=in_ap)
        self.ninst += 1
        ds.cnt += 16
        ins.then_inc(ds.sem, 16)
        self.trace[qn].append(("i", ds.sem.num, 16))
        d = Dep(ds.sem, ds.cnt, "dma")
        for b in reads:
            b.r["dma:" + ds.name] = d
        for b in writes:
            b.w = d
            b.r = {}
        return ins

    def barrier(self, only=None, skip=("pool",)):
        targets = []
        for e in self.E.values():
            assert not e.pending, "pending instruction on %s at barrier" % e.name
            if e.cnt > 0:
                targets.append((e.name, e.sem, e.cnt))
        for ds in self.dsems:
            if ds.cnt > 0:
                targets.append(("dma", ds.sem, ds.cnt))
        for e in self.E.values():
            if only is not None and e.name not in only:
                continue
            if only is None and e.name in skip:
                continue
            for (src, sem, val) in targets:
                if src == e.name:
                    continue
                if e.known.get(sem, 0) >= val:
                    continue
                e.h.wait_ge(sem, val)
                e.known[sem] = val
                self.ninst += 1
                self.trace[e.name].append(("w", sem.num, val))

    def check_deadlock(self):
        val = {}
        ptr = {k: 0 for k in self.trace}
        progress = True
        while progress:
            progress = False
            for k, tr in self.trace.items():
                while ptr[k] < len(tr):
                    kind, sname, v = tr[ptr[k]]
                    if kind == "w":
                        if val.get(sname, 0) >= v:
                            ptr[k] += 1
                            progress = True
                        else:
                            break
                    else:
                        val[sname] = val.get(sname, 0) + v
                        ptr[k] += 1
                        progress = True
        stuck = {k: (ptr[k], len(tr), tr[ptr[k]], val.get(tr[ptr[k]][1], 0)) for k, tr in self.trace.items() if ptr[k] < len(tr)}
        return stuck


C_SBQ, C_SBK, C_SBV = 0, 512, 1024
C_MQ, C_CKV, C_KR = 1536, 2304, 2816
C_RQ, C_RK, C_RV, C_RG = 2880, 3392, 3904, 4416
C_LX, C_LY = 4928, 5440


def _win_jobs():
    jobs = []
    ar = np.arange
    jobs.append(("sbv0", ar(C_SBV, C_SBV + 256)))
    jobs.append(("sbv1", ar(C_SBV + 256, C_SBV + 512)))
    for h in range(4):
        jobs.append(("sbqk%d" % h, np.concatenate([ar(C_SBQ + 128 * h, C_SBQ + 128 * h + 128),
                                                   ar(C_SBK + 128 * h, C_SBK + 128 * h + 128)])))
    jobs.append(("ckv0", ar(C_CKV, C_CKV + 256)))
    jobs.append(("ckv1", ar(C_CKV + 256, C_CKV + 512)))
    jobs.append(("kr", ar(C_KR, C_KR + 64)))
    for h in range(4):
        jobs.append(("mq%d" % h, ar(C_MQ + 192 * h, C_MQ + 192 * h + 192)))
    jobs.append(("rv0", ar(C_RV, C_RV + 256)))
    jobs.append(("rv1", ar(C_RV + 256, C_RV + 512)))
    jobs.append(("rg0", ar(C_RG, C_RG + 256)))
    jobs.append(("rg1", ar(C_RG + 256, C_RG + 512)))
    for h in range(4):
        jobs.append(("rqk%d" % h, np.concatenate([ar(C_RQ + 128 * h, C_RQ + 128 * h + 128),
                                                  ar(C_RK + 128 * h, C_RK + 128 * h + 128)])))
    for c in range(4):
        jobs.append(("lxy%d" % c, np.concatenate([ar(C_LX + 128 * c, C_LX + 128 * c + 128),
                                                  ar(C_LY + 128 * c, C_LY + 128 * c + 128)])))
    return jobs


WIN_JOBS = _win_jobs()
WIN_OFF = {}
_o = 0
for _n, _c in WIN_JOBS:
    WIN_OFF[_n] = (_o, len(_c))
    _o += KC * len(_c)
WIN_TOT = _o


def _pack_cols(W, cols):
    K = W.shape[0]
    sub = W[:, cols]
    return np.ascontiguousarray(sub.reshape(K // 128, 128, len(cols)).transpose(1, 0, 2)).reshape(128, -1)


def _tile_cols(W, tw):
    K, Fd = W.shape
    return np.ascontiguousarray(W.reshape(K // 128, 128, Fd // tw, tw).transpose(2, 1, 0, 3)).reshape(Fd // tw, 128, -1)


def _ukv_pack(Wukv):
    blocks = []
    for h in range(4):
        blocks.append(_pack_cols(Wukv, np.arange(h * 256, h * 256 + 128)))
    vcols = np.concatenate([np.arange(h * 256 + 128, h * 256 + 256) for h in range(4)])
    blocks.append(_pack_cols(Wukv, vcols[:256]))
    blocks.append(_pack_cols(Wukv, vcols[256:]))
    return np.concatenate(blocks, axis=1)


PV_LN1, PV_LN2, PV_KVG, PV_CW, PV_CB, PV_BA, PV_BX, PV_LAM = 0, 16, 32, 36, 52, 56, 60, 64
NPV = 68

CC_FM, CC_FR, CC_KDEC, CC_ONE, CC_EPS = 0, 1, 2, 6, 7
NCC = 8
CM_ID, CM_R64, CM_R128, CM_LT, CM_NEG, CM_DEC, CM_QDEC = 0, 1, 2, 3, 4, 5, 9
NCM = 13


def _consts():
    cc = np.zeros((128, NCC), np.float32)
    p = np.arange(128)
    cc[:, CC_FM] = (THETA ** (-(2.0 * (p % 32)) / 64.0)).astype(np.float32)
    cc[:, CC_FR] = (THETA ** (-(2.0 * (p % 64)) / 128.0)).astype(np.float32)
    gam = 1.0 - 2.0 ** (-5.0 - np.arange(4, dtype=np.float64))
    lg = np.log(gam)
    for h in range(4):
        cc[:, CC_KDEC + h] = np.exp(lg[h] * (127.0 - p)).astype(np.float32)
    cc[:, CC_ONE] = 1.0
    cc[:, CC_EPS] = EPS
    cm = np.zeros((NCM, 128, 128), np.float32)
    cm[CM_ID] = np.eye(128, dtype=np.float32)
    for m in range(64):
        if m < 32:
            cm[CM_R64, m + 32, m] = -1.0
        else:
            cm[CM_R64, m - 32, m] = 1.0
    for m in range(128):
        if m < 64:
            cm[CM_R128, m + 64, m] = -1.0
        else:
            cm[CM_R128, m - 64, m] = 1.0
    tt = np.arange(128)[:, None]
    ss = np.arange(128)[None, :]
    cm[CM_LT] = (ss < tt).astype(np.float32)
    cm[CM_NEG] = np.where(ss <= tt, 0.0, -1.0e30).astype(np.float32)
    for h in range(4):
        dq = (ss - tt).astype(np.float64)
        cm[CM_DEC + h] = np.where(dq >= 0, np.exp(lg[h] * np.maximum(dq, 0.0)), 0.0).astype(np.float32)
        cm[CM_QDEC + h] = np.broadcast_to(np.exp(lg[h] * (np.arange(128) + 1.0))[None, :], (128, 128)).astype(np.float32)
    cd = [float(np.exp(lg[h] * 128.0)) for h in range(4)]
    return cc, cm, cd


def _col128(v):
    return np.ascontiguousarray(v.reshape(-1, 128).T)


def prep_weights(inp, layers):
    out = {}
    NL = len(layers)
    winp = np.empty((NL, 128, WIN_TOT), np.float32)
    ukv = np.empty((NL, 128, 4096), np.float32)
    wax = np.empty((NL, 128, 1024), np.float32)
    wbr = np.empty((NL, 16, 128, 2048), np.float32)
    wgt = np.empty((NL, 64, 128, 2048), np.float32)
    wout = np.empty((NL, 16, 128, 2048), np.float32)
    wfgu = np.empty((NL, FC, 128, 4096), np.float32)
    wfd = np.empty((NL, 16, 128, FC * 128), np.float32)
    pv = np.zeros((NL, 128, NPV), np.float32)
    for i, l in enumerate(layers):
        W = inp["w_in"][l]
        for n, cols in WIN_JOBS:
            off, nc_ = WIN_OFF[n]
            winp[i, :, off:off + KC * nc_] = _pack_cols(W, cols)
        ukv[i] = _ukv_pack(inp["mla_w_ukv"][l])
        wa = inp["lru_w_a"][l]
        wx = inp["lru_w_x"][l]
        wax[i, :, 0:512] = wa.transpose(1, 0, 2).reshape(128, 512)
        wax[i, :, 512:1024] = wx.transpose(1, 0, 2).reshape(128, 512)
        wb = inp["w_branch"][l].reshape(4, 4, 128, 16, 128)
        wbr[i] = wb.transpose(3, 2, 0, 1, 4).reshape(16, 128, 2048)
        wg = inp["w_gate"][l].reshape(4, 16, 128, 16, 128)
        wgt[i] = wg.transpose(3, 0, 2, 1, 4).reshape(64, 128, 2048)
        wout[i] = _tile_cols(inp["w_out"][l], 128)
        g = _tile_cols(inp["ffn_w_gate"][l], 128)
        u = _tile_cols(inp["ffn_w_up"][l], 128)
        wfgu[i, :, :, 0:2048] = g
        wfgu[i, :, :, 2048:4096] = u
        wfd[i] = _tile_cols(inp["ffn_w_down"][l], 128)
        pv[i, :, PV_LN1:PV_LN1 + 16] = _col128(inp["ln1"][l])
        pv[i, :, PV_LN2:PV_LN2 + 16] = _col128(inp["ln2"][l])
        pv[i, :, PV_KVG:PV_KVG + 4] = _col128(inp["mla_kv_gain"][l])
        cw = inp["lru_conv_w"][l]
        for tap in range(4):
            pv[i, :, PV_CW + 4 * tap:PV_CW + 4 * tap + 4] = _col128(cw[tap])
        pv[i, :, PV_CB:PV_CB + 4] = _col128(inp["lru_conv_b"][l])
        pv[i, :, PV_BA:PV_BA + 4] = _col128(inp["lru_b_a"][l])
        pv[i, :, PV_BX:PV_BX + 4] = _col128(inp["lru_b_x"][l])
        pv[i, :, PV_LAM:PV_LAM + 4] = _col128(inp["lru_lambda"][l])
    out.update(winp=winp, ukv=ukv, wax=wax, wbr=wbr, wgt=wgt, wout=wout, wfgu=wfgu, wfd=wfd, pv=pv)
    return out


def build(NL, final_norm=True, dbg=False, stages=("mix", "A", "B", "C", "D", "gate", "wout", "ffn")):
    nc = bass.Bass("TRN2", target_bir_lowering=False)
    cc_np, cm_np, CD = _consts()

    def din(name, shape, dt=F32):
        return nc.dram_tensor(name, list(shape), dt, kind="ExternalInput").ap()

    xT_in = din("xT", [D, T])
    pos_in = din("pos", [1, T], I32)
    lnf_in = din("lnf", [128, 16])
    cc_in = din("cc", [128, NCC])
    cm_in = din("cm", [NCM, 128, 128])
    winp = din("winp", [NL, 128, WIN_TOT])
    ukv = din("ukv", [NL, 128, 4096])
    wax = din("wax", [NL, 128, 1024])
    wbr = din("wbr", [NL, 16, 128, 2048])
    wgt = din("wgt", [NL, 64, 128, 2048])
    wout = din("wout", [NL, 16, 128, 2048])
    wfgu = din("wfgu", [NL, FC, 128, 4096])
    wfd = din("wfd", [NL, 16, 128, FC * 128])
    pv_in = din("pv", [NL, 128, NPV])
    outT = nc.dram_tensor("outT", [D, T], F32, kind="ExternalOutput").ap()
    skind = "ExternalOutput" if dbg else "Internal"
    xres = nc.dram_tensor("xres", [D, T], F32, kind=skind).ap()
    obT = nc.dram_tensor("obT", [2048, T], BF16, kind=skind).ap()
    mixT = nc.dram_tensor("mixT", [D, T], BF16, kind=skind).ap()
    sgD = nc.dram_tensor("sgD", [4, D, T], BF16, kind="Internal").ap()

    with ExitStack() as st:
        S = Sy(nc, st)

        uid = [0]

        def sb(name, shape, dt, stack=st):
            uid[0] += 1
            return stack.enter_context(nc.sbuf_tensor("%s_u%d" % (name, uid[0]), list(shape), dt))

        hbuf = sb("hbuf", [128, KC, T], BF16)
        Bh = Buf("hbuf")
        WSL = 6144
        wslots = []
        for i in range(3):
            wslots.append((sb("wslot%d" % i, [128, WSL], BF16), Buf("wslot%d" % i), S.dsem("wld%d" % i)))
        wctr = [0]
        ccs = sb("ccs", [128, NCC], F32)
        pvs = sb("pvs", [128, NPV], F32)
        lnfs = sb("lnfs", [128, 16], F32)
        Bcc, Bpv = Buf("ccs"), Buf("pvs")
        ident_f = sb("ident_f", [128, 128], F32)
        cmb = sb("cmb", [128, 5, 128], BF16)
        cmf = sb("cmf", [128, 9, 128], F32)
        Bcm = Buf("cm")
        ones_bf = sb("ones_bf", [128, 128], BF16)
        Bones = Buf("ones")
        Btab = Buf("tabs")
        ps = [st.enter_context(nc.psum_tensor("ps%d" % i, [128, 512], F32)) for i in range(8)]
        PB = [Buf("ps%d" % i) for i in range(8)]
        lin_ctr = [0]
        lin_nb = [4]
        ev_ctr = [0]
        misc_ds = S.dsem("misc")
        out_ds = S.dsem("outst")
        ODS = [S.dsem("ost%d" % i) for i in range(2)]
        GWD = [S.dsem("gwl%d" % i) for i in range(3)]
        GWB = [Buf("gw%d" % i) for i in range(3)]
        SGD = [S.dsem("sgst%d" % i) for i in range(4)]
        sgs = [(sb("sgs%d" % i, [128, 512], BF16), Buf("sgs%d" % i)) for i in range(4)]
        gbanks = [[0, 1, 2, 3]]
        XDS = [S.dsem("oxl%d" % i) for i in range(2)]
        FDS = [S.dsem("fxl%d" % i) for i in range(2)]
        NDS = [S.dsem("nxl%d" % i) for i in range(3)]
        NSD = [S.dsem("nxs%d" % i) for i in range(3)]
        WBDS = [S.dsem("wbs%d" % i) for i in range(4)]
        tabD = nc.dram_tensor("tabD", [4, 128, T], BF16, kind="Internal").ap()

        ident_bf = cmb[:, 0, :]
        r64_bf = cmb[:, 1, :]
        r128_bf = cmb[:, 2, :]
        lt_bf = cmb[:, 3, :]

        def lt_f():
            return cmf_lt[:]

        wl_n = [3]

        def wload(src2d, nelem):
            i = wctr[0] % wl_n[0]
            wctr[0] += 1
            t, b, ds = wslots[i]
            S.dma("pool", ds, t[:, 0:nelem], src2d, writes=[b])
            return t, b

        def lin_bank(n=None):
            n = lin_nb[0] if n is None else n
            b = lin_ctr[0] % n
            lin_ctr[0] += 1
            return b

        def evac(out_ap, in_ap, reads, writes, scale=None):
            use_act = (ev_ctr[0] % 2 == 0)
            ev_ctr[0] += 1
            import os
            if "evact" in os.environ.get("K_DBG", ""):
                use_act = True
            if "evdve" in os.environ.get("K_DBG", ""):
                use_act = False
            if use_act:
                if scale is None:
                    S.op("act", lambda e: e.activation(out_ap, in_ap, AF.Copy), reads, writes)
                else:
                    S.op("act", lambda e: e.activation(out_ap, in_ap, AF.Copy, scale=scale), reads, writes)
            else:
                if scale is None:
                    S.op("dve", lambda e: e.tensor_copy(out_ap, in_ap), reads, writes)
                else:
                    S.op("dve", lambda e: e.tensor_scalar(out_ap, in_ap, scale, None, ALU.mult), reads, writes)

        def mm(out_ap, lhsT, rhs, start, stop, reads, writes, inc=None):
            S.op("pe", lambda e: e.matmul(out_ap, lhsT, rhs, start=start, stop=stop), reads, writes, inc=(stop if inc is None else inc))

        def lin_fm(wt, wb, col0, M, nk, rhs_fn, rhs_bufs, epi, ncols_job, tgs=range(4), N=512):
            wv = wt[:, 0:nk * ncols_job].rearrange("p (c n) -> p c n", n=ncols_job)
            for tg in tgs:
                bk = lin_bank()
                for kc in range(nk):
                    mm(ps[bk][0:M, 0:N], wv[:, kc, col0:col0 + M], rhs_fn(kc, tg), kc == 0, kc == nk - 1,
                       [wb] + rhs_bufs, [PB[bk]])
                epi(tg, bk)

        def lin_tm(wt, wb, col0, Ncol, nk, lhs_fn, lhs_bufs, epi, ncols_job):
            wv = wt[:, 0:nk * ncols_job].rearrange("p (c n) -> p c n", n=ncols_job)
            import os
            dbgm = os.environ.get("K_DBG", "")
            for tile in range(NT):
                bk = lin_bank()
                for kc in range(nk):
                    if "nomm" in dbgm and kc > 0:
                        continue
                    mm(ps[bk][:, 0:Ncol], lhs_fn(kc, tile), wv[:, kc, col0:col0 + Ncol], kc == 0, (kc == nk - 1) or ("nomm" in dbgm),
                       [wb] + lhs_bufs, [PB[bk]])
                if "noevac" not in dbgm:
                    epi(tile, bk)

        def h_rhs(kc, tg):
            return hbuf[:, kc, tg * 512:(tg + 1) * 512]

        def h_lhs(kc, tile):
            return hbuf[:, kc, tile * 128:(tile + 1) * 128]

        S.dma("sp", misc_ds, ccs[:], cc_in, writes=[Bcc])
        S.dma("sp", misc_ds, ident_f[:], cm_in[CM_ID], writes=[Bcm])
        S.dma("sp", misc_ds, lnfs[:], lnf_in, writes=[Bcc])
        cmf_lt = sb("cmf_lt", [128, 128], F32)
        S.dma("sp", misc_ds, cmf_lt[:], cm_in[CM_LT], writes=[Bcm])
        S.dma("sp", misc_ds, cmf[:], cm_in[CM_NEG:CM_NEG + 9].rearrange("n p j -> p n j"), writes=[Bcm])
        S.dma("pool", misc_ds, cmb[:, 0:4, :], cm_in[CM_ID:CM_ID + 4].rearrange("n p j -> p n j"), writes=[Bcm])
        S.op("dve", lambda e: e.memset(ones_bf[:], 1.0), writes=[Bones])
        notlt = sb("notlt", [128, 128], F32)
        S.barrier()

        S.op("dve", lambda e: e.tensor_scalar(notlt[:], cmf_lt[:], -1.0, 1.0, ALU.mult, ALU.add), [Bcm], [Bcm])
        with ExitStack() as ts:
            posi = sb("posi", [128, T], I32, ts)
            posf = sb("posf", [128, T], F32, ts)
            ang = sb("ang", [128, T], F32, ts)
            kf = sb("kf", [128, T], F32, ts)
            ki = sb("ki", [128, T], I32, ts)
            Bt = [Buf("t%d" % i) for i in range(5)]
            pos_b = bass.AP(pos_in.tensor, pos_in.offset, [[0, 128], [1, T]])
            S.dma("sp", misc_ds, posi[:], pos_b, writes=[Bt[0]])
            S.op("dve", lambda e: e.tensor_copy(posf[:], posi[:]), [Bt[0]], [Bt[1]])
            tabs = sb("tabs", [128, T], BF16, ts)
            for (fcol, ci, si) in ((CC_FM, 0, 1), (CC_FR, 2, 3)):
                for shift, ti in ((0.0, si), (0.5 * math.pi, ci)):
                    tab = tabs
                    S.op("dve", lambda e: e.tensor_scalar(ang[:], posf[:], ccs[:, fcol:fcol + 1], None, ALU.mult),
                         [Bt[1], Bcc], [Bt[2]])
                    if shift != 0.0:
                        S.op("dve", lambda e: e.tensor_scalar(ang[:], ang[:], shift, None, ALU.add), [Bt[2]], [Bt[2]])
                    S.op("dve", lambda e: e.tensor_scalar(kf[:], ang[:], 1.0 / TWO_PI, None, ALU.mult), [Bt[2]], [Bt[3]])
                    S.op("dve", lambda e: e.tensor_copy(ki[:], kf[:]), [Bt[3]], [Bt[4]])
                    S.op("dve", lambda e: e.tensor_copy(kf[:], ki[:]), [Bt[4]], [Bt[3]])
                    c1 = 6.28125
                    c2 = TWO_PI - c1
                    S.op("dve", lambda e: e.scalar_tensor_tensor(ang[:], kf[:], -c1, ang[:], ALU.mult, ALU.add), [Bt[3], Bt[2]], [Bt[2]])
                    S.op("dve", lambda e: e.scalar_tensor_tensor(ang[:], kf[:], -c2, ang[:], ALU.mult, ALU.add), [Bt[3], Bt[2]], [Bt[2]])
                    S.op("dve", lambda e: e.tensor_scalar(ang[:], ang[:], math.pi, -math.pi, ALU.min, ALU.max), [Bt[2]], [Bt[2]])
                    S.op("act", lambda e: e.activation(tab[:], ang[:], AF.Sin), [Bt[2]], [Btab])
                    S.dma("sp", misc_ds, tabD[ti], tab[:], reads=[Btab])
            S.barrier()

        def norm_phase(src, gsb, gcol, final):
            G = 256 if final else 512
            nb = 3 if final else 2
            with ExitStack() as ns:
                xb = [sb("nx%d" % i, [128, KC, G], F32, ns) for i in range(nb)]
                Bx = [Buf("nx%d" % i) for i in range(nb)]
                sq = [sb("nsq%d" % i, [128, G], BF16, ns) for i in range(2)]
                Bsq = [Buf("nsq%d" % i) for i in range(2)]
                rsb = sb("nrs", [128, G], F32, ns)
                Brs = Buf("nrs")
                srcv = src.rearrange("(c p) t -> p c t", p=128)
                ngrp = T // G

                def load_grp(g_):
                    for hh in range(4):
                        S.dma("sp", NDS[g_ % nb], xb[g_ % nb][:, hh * 4:(hh + 1) * 4, :], srcv[:, hh * 4:(hh + 1) * 4, g_ * G:(g_ + 1) * G],
                              writes=[Bx[g_ % nb]])
                for g_ in range(min(nb - 1, ngrp)):
                    load_grp(g_)
                for tq in range(ngrp):
                    x_, bx = xb[tq % nb], Bx[tq % nb]
                    tsl = slice(tq * G, (tq + 1) * G)
                    if tq + nb - 1 < ngrp:
                        load_grp(tq + nb - 1)
                    bk = lin_bank()
                    for c in range(KC):
                        s_, bs = sq[c % 2], Bsq[c % 2]
                        S.op("act", lambda e: e.activation(s_[:], x_[:, c, :], AF.Square), [bx], [bs])
                        mm(ps[bk][:, 0:G], ones_bf[:, 0:128], s_[:], c == 0, c == KC - 1, [Bones, bs], [PB[bk]], inc=True)
                    S.op("act", lambda e: e.activation(rsb[:], ps[bk][:, 0:G], AF.Sqrt, bias=ccs[:, CC_EPS:CC_EPS + 1], scale=1.0 / D),
                         [PB[bk], Bcc], [Brs])
                    S.op("dve", lambda e: e.reciprocal(rsb[:], rsb[:]), [Brs], [Brs])
                    for c in range(KC):
                        if final:
                            S.op("dve", lambda e: e.scalar_tensor_tensor(x_[:, c, :], x_[:, c, :], gsb[:, gcol + c:gcol + c + 1], rsb[:],
                                                                          ALU.mult, ALU.mult), [bx, Brs, Bpv, Bcc], [bx])
                        else:
                            S.op("dve", lambda e: e.scalar_tensor_tensor(hbuf[:, c, tsl], x_[:, c, :],
                                                                          gsb[:, gcol + c:gcol + c + 1], rsb[:], ALU.mult, ALU.mult),
                                 [bx, Brs, Bpv, Bcc], [Bh])
                    if final:
                        ov = outT.rearrange("(c p) t -> p c t", p=128)
                        for hh in range(4):
                            S.dma("sp", NSD[tq % nb], ov[:, hh * 4:(hh + 1) * 4, tsl], x_[:, hh * 4:(hh + 1) * 4, :], reads=[bx])
                S.barrier()

        def rope_fm(bk, R, ctab, stab, rmat, scale, dst_ap, dst_buf, tg, tmp):
            xs, t1, t2, Bxs, Bt1, Bt2 = tmp
            sl = slice(tg * 512, (tg + 1) * 512)
            S.op("act", lambda e: e.activation(xs[0:R, :], ps[bk][0:R, :], AF.Copy, scale=scale), [PB[bk]], [Bxs])
            b2 = lin_bank()
            mm(ps[b2][0:R, :], rmat[0:R, 0:R], xs[0:R, :], True, True, [Bcm, Bxs], [PB[b2]])
            S.op("dve", lambda e: e.tensor_tensor(t1[0:R, :], xs[0:R, :], ctab[0:R, sl], ALU.mult), [Bxs, Btab], [Bt1])
            S.op("dve", lambda e: e.tensor_tensor(t2[0:R, :], ps[b2][0:R, :], stab[0:R, sl], ALU.mult), [PB[b2], Btab], [Bt2])
            S.op("dve", lambda e: e.tensor_tensor(dst_ap, t1[0:R, :], t2[0:R, :], ALU.add), [Bt1, Bt2], [dst_buf])

        def attn_core(mode, qT, Bq, kT, Bk, vall, Bv, h, W, oT, Bo, qpe=None, kpe=None, Bpe=None, Bqpe=None, ones_big=None, Bonesb=None, tickf=None):
            e1, zc, Pb, wb_, wT, col, dg, Be1, Bzc, BPb, Bw, BwT, Bcol, Bdg = W
            for i in range(NT if "att1" not in stages else 1):
                L = 128 * (i + 1)
                nch = (L + 511) // 512
                for ch in range(nch):
                    cw = min(512, L - ch * 512)
                    bk = 4 + (ch % 2)
                    cs = slice(ch * 512, ch * 512 + cw)
                    mm(ps[bk][:, 0:cw], qT[:, i * 128:(i + 1) * 128], kT[:, cs], True, mode == "sb", [Bq, Bk], [PB[bk]])
                    if mode == "mla":
                        mm(ps[bk][:, 0:cw], qpe[0:64, i * 128:(i + 1) * 128], kpe[0:64, cs], False, True, [Bqpe, Bpe], [PB[bk]])
                    if mode == "sb":
                        S.op("act", lambda e: e.activation(e1[:, cs], ps[bk][:, 0:cw], AF.Exp), [PB[bk]], [Be1])
                        S.op("dve", lambda e: e.tensor_copy(zc[:, cs], ps[bk][:, 0:cw]), [PB[bk]], [Bzc])
                    else:
                        evac(zc[:, cs], ps[bk][:, 0:cw], [PB[bk]], [Bzc])
                dsl = slice(L - 128, L)
                if tickf is not None:
                    tickf()
                if mode == "sb":
                    S.op("act", lambda e: e.activation(e1[:, 0:L], e1[:, 0:L], AF.Ln, bias=ccs[:, CC_ONE:CC_ONE + 1]), [Be1, Bcc], [Be1])
                    S.op("dve", lambda e: e.tensor_tensor(e1[:, dsl], e1[:, dsl], cmf_lt[:], ALU.mult), [Be1, Bcm], [Be1])
                    S.op("dve", lambda e: e.tensor_tensor_scan(Pb[:, 0:L], ones_big[:, 0:L], e1[:, 0:L], 0.0, ALU.mult, ALU.subtract),
                         [Bonesb, Be1], [BPb])
                    S.op("dve", lambda e: e.tensor_tensor(zc[:, 0:L], zc[:, 0:L], e1[:, 0:L], ALU.subtract), [Bzc, Be1], [Bzc])
                    S.op("dve", lambda e: e.tensor_tensor(zc[:, 0:L], zc[:, 0:L], Pb[:, 0:L], ALU.subtract), [Bzc, BPb], [Bzc])
                    S.op("act", lambda e: e.activation(wb_[:, 0:L], zc[:, 0:L], AF.Exp, bias=Pb[:, L - 1:L]), [Bzc, BPb], [Bw])
                    S.op("dve", lambda e: e.tensor_tensor(wb_[:, dsl], wb_[:, dsl], lt_bf, ALU.mult), [Bw, Bcm], [Bw])
                    rhsT, BrT = ident_bf, Bcm
                else:
                    S.op("dve", lambda e: e.tensor_tensor(zc[:, dsl], zc[:, dsl], cmf[:, 0, :], ALU.add), [Bzc, Bcm], [Bzc])
                    S.op("dve", lambda e: e.tensor_reduce(col[:, 0:1], zc[:, 0:L], AX.X, ALU.max, negate=True), [Bzc], [Bcol])
                    S.op("act", lambda e: e.activation(wb_[:, 0:L], zc[:, 0:L], AF.Exp, bias=col[:, 0:1], accum_out=col[:, 1:2]),
                         [Bzc, Bcol], [Bw, Bcol])
                    S.op("dve", lambda e: e.reciprocal(col[:, 2:3], col[:, 1:2]), [Bcol], [Bcol])
                    S.op("dve", lambda e: e.tensor_scalar(dg[:], ident_f[:], col[:, 2:3], None, ALU.mult), [Bcm, Bcol], [Bdg])
                    rhsT, BrT = dg[:], Bdg
                kb = 0
                while kb <= i:
                    n4 = min(4, i + 1 - kb)
                    for j in range(n4):
                        S.op("pe", lambda e: e.matmul(ps[6][:, j * 128:(j + 1) * 128], wb_[:, (kb + j) * 128:(kb + j + 1) * 128], rhsT,
                                                      start=True, stop=True), [Bw, BrT], [PB[6]], inc=(j == n4 - 1))
                    evac(wT[:, kb * 128:(kb + n4) * 128], ps[6][:, 0:n4 * 128], [PB[6]], [BwT])
                    kb += n4
                for kb in range(i + 1):
                    mm(ps[7][:, 0:128], vall[:, kb, h * 128:(h + 1) * 128], wT[:, kb * 128:(kb + 1) * 128], kb == 0, kb == i,
                       [Bv, BwT], [PB[7]])
                evac(oT[:, i * 128:(i + 1) * 128], ps[7][:, 0:128], [PB[7]], [Bo])

        for l in range(NL):
            S.dma("sp", misc_ds, pvs[:], pv_in[l], writes=[Bpv])

            def wjob(name):
                off, ncj = WIN_OFF[name]
                t, b = wload(winp[l, :, off:off + KC * ncj], KC * ncj)
                return t, b, ncj

            if "mix" in stages:
                norm_phase(xT_in if l == 0 else xres, pvs, PV_LN1, False)
                wl_n[0] = 3

                def gate_gen():
                    gt = wslots[2][0]
                    u = 0
                    for m in range(16):
                        for b in range(4):
                            j = (m * 4 + b) % 3
                            gw = gt[:, j * 2048:(j + 1) * 2048]
                            S.dma("pool", GWD[j], gw, wgt[l, m * 4 + b], writes=[GWB[j]])
                            gv = gw.rearrange("p (c j) -> p c j", j=128)
                            for tg in range(4):
                                gb = gbanks[0]
                                gate_flush(len(gb) - 1)
                                bk = gb[u % len(gb)]
                                for kc in range(KC):
                                    mm(ps[bk][:, :], gv[:, kc, :], hbuf[:, kc, tg * 512:(tg + 1) * 512], kc == 0, kc == KC - 1,
                                       [GWB[j], Bh], [PB[bk]])
                                gpend.append((bk, m, b, tg, u))
                                u += 1
                                yield
                    gate_flush()
                    yield

                gpend = []

                def gate_flush(keep=0):
                    while len(gpend) > keep:
                        gate_fin(*gpend.pop(0))

                def gate_fin(bk, m, b, tg, u):
                    st_, Bst = sgs[u % 4]
                    S.op("act", lambda e: e.activation(st_[:], ps[bk][:, :], AF.Sigmoid), [PB[bk]], [Bst])
                    S.dma("sp", SGD[u % 4], sgD[b, m * 128:(m + 1) * 128, tg * 512:(tg + 1) * 512], st_[:], reads=[Bst])

                ggen = gate_gen() if "gate" in stages else iter(())

                def tick(n=1):
                    for _ in range(n):
                        next(ggen, None)

                if "A" in stages:
                    with ExitStack() as bs:
                        vall = sb("a_v", [128, NT, 512], BF16, bs)
                        Bv = Buf("a_v")
                        ctxs = []
                        for c in range(2):
                            cx = dict(
                                qT=sb("a_q%d" % c, [128, T], BF16, bs), Bq=Buf("a_q%d" % c),
                                kT=sb("a_k%d" % c, [128, T], BF16, bs), Bk=Buf("a_k%d" % c),
                                oT=sb("a_o%d" % c, [128, T], BF16, bs), Bo=Buf("a_o%d" % c),
                                bt=sb("a_bt%d" % c, [128, T], BF16, bs), Bbt=Buf("a_bt%d" % c),
                                ob=sb("a_ob%d" % c, [128, T], F32, bs), Bob=Buf("a_ob%d" % c),
                                R=sb("a_R%d" % c, [128, T], BF16, bs), BR=Buf("a_R%d" % c),
                                w=sb("a_w%d" % c, [128, T], BF16, bs), Bw=Buf("a_w%d" % c),
                                wT=sb("a_wT%d" % c, [128, T], BF16, bs), BwT=Buf("a_wT%d" % c),
                                zb=(0, 1) if c == 0 else (4, 5), tb=2 if c == 0 else 6, obk=3 if c == 0 else 7)
                            ctxs.append(cx)
                        for half in range(2):
                            t, b, ncj = wjob("sbv%d" % half)

                            def epi_v(tile, bk, half=half):
                                evac(vall[:, tile, half * 256:(half + 1) * 256], ps[bk][:, 0:256], [PB[bk]], [Bv])
                            lin_tm(t, b, 0, 256, KC, h_lhs, [Bh], epi_v, ncj)

                        def rev(t_, L):
                            ap = t_[:, 0:L]
                            return bass.AP(ap.tensor, ap.offset + (L - 1), [list(ap.ap[0]), [-1, L]])

                        def sb_s1(cx, i):
                            L = 128 * (i + 1)
                            nch = (L + 511) // 512
                            for ch in range(nch):
                                cw = min(512, L - ch * 512)
                                bk = cx["zb"][ch % 2]
                                cs = slice(ch * 512, ch * 512 + cw)
                                mm(ps[bk][:, 0:cw], cx["qT"][:, i * 128:(i + 1) * 128], cx["kT"][:, cs], True, True, [cx["Bq"], cx["Bk"]], [PB[bk]])
                                S.op("act", lambda e: e.activation(cx["bt"][:, cs], ps[bk][:, 0:cw], AF.Sigmoid), [PB[bk]], [cx["Bbt"]])
                                S.op("act", lambda e: e.activation(cx["ob"][:, cs], ps[bk][:, 0:cw], AF.Sigmoid, scale=-1.0), [PB[bk]], [cx["Bob"]])
                            dsl = slice(L - 128, L)
                            S.op("dve", lambda e: e.tensor_tensor(cx["bt"][:, dsl], cx["bt"][:, dsl], lt_bf, ALU.mult), [cx["Bbt"], Bcm], [cx["Bbt"]])
                            S.op("dve", lambda e: e.tensor_tensor(cx["ob"][:, dsl], cx["ob"][:, dsl], cmf_lt[:], ALU.mult), [cx["Bob"], Bcm], [cx["Bob"]])
                            S.op("dve", lambda e: e.tensor_tensor(cx["ob"][:, dsl], cx["ob"][:, dsl], notlt[:], ALU.add), [cx["Bob"], Bcm], [cx["Bob"]])

                        def sb_s2(cx, i):
                            L = 128 * (i + 1)
                            S.op("dve", lambda e: e.tensor_tensor_scan(rev(cx["R"], L), rev(cx["ob"], L), rev(cx["ob"], L), 1.0, ALU.mult, ALU.bypass),
                                 [cx["Bob"]], [cx["BR"]])
                            S.op("dve", lambda e: e.tensor_tensor(cx["w"][:, 0:L - 1], cx["bt"][:, 0:L - 1], cx["R"][:, 1:L], ALU.mult),
                                 [cx["Bbt"], cx["BR"]], [cx["Bw"]])
                            S.op("dve", lambda e: e.tensor_copy(cx["w"][:, L - 1:L], cx["bt"][:, L - 1:L]), [cx["Bbt"]], [cx["Bw"]])

                        def sb_s4(cx, i):
                            kb = 0
                            tb = cx["tb"]
                            while kb <= i:
                                n4 = min(4, i + 1 - kb)
                                for j in range(n4):
                                    S.op("pe", lambda e: e.matmul(ps[tb][:, j * 128:(j + 1) * 128], cx["w"][:, (kb + j) * 128:(kb + j + 1) * 128], ident_bf,
                                                                  start=True, stop=True), [cx["Bw"], Bcm], [PB[tb]], inc=(j == n4 - 1))
                                S.op("act", lambda e: e.activation(cx["wT"][:, kb * 128:(kb + n4) * 128], ps[tb][:, 0:n4 * 128], AF.Copy), [PB[tb]], [cx["BwT"]])
                                kb += n4

                        def sb_s5(cx, i, h):
                            ok = cx["obk"]
                            for kb in range(i + 1):
                                mm(ps[ok][:, 0:128], vall[:, kb, h * 128:(h + 1) * 128], cx["wT"][:, kb * 128:(kb + 1) * 128], kb == 0, kb == i,
                                   [Bv, cx["BwT"]], [PB[ok]])
                            S.op("dve", lambda e: e.tensor_copy(cx["oT"][:, i * 128:(i + 1) * 128], ps[ok][:, 0:128]), [PB[ok]], [cx["Bo"]])

                        for pair in range(2 if "A_v" not in stages else 0):
                            for c in range(2):
                                h = 2 * pair + c
                                cx = ctxs[c]
                                t, b, ncj = wjob("sbqk%d" % h)

                                def epi_q(tg, bk, cx=cx):
                                    evac(cx["qT"][:, tg * 512:(tg + 1) * 512], ps[bk][:, :], [PB[bk]], [cx["Bq"]], scale=128.0 ** -0.5)

                                def epi_k(tg, bk, cx=cx):
                                    evac(cx["kT"][:, tg * 512:(tg + 1) * 512], ps[bk][:, :], [PB[bk]], [cx["Bk"]])
                                lin_fm(t, b, 0, 128, KC, h_rhs, [Bh], epi_q, ncj)
                                lin_fm(t, b, 128, 128, KC, h_rhs, [Bh], epi_k, ncj)
                            items = [(c, i) for i in range(NT if "att1" not in stages else 1) for c in range(2)]
                            nit = len(items)
                            for k in range(nit + 3):
                                if k < nit:
                                    sb_s1(ctxs[items[k][0]], items[k][1])
                                if 0 <= k - 1 < nit:
                                    sb_s2(ctxs[items[k - 1][0]], items[k - 1][1])
                                if 0 <= k - 2 < nit:
                                    sb_s4(ctxs[items[k - 2][0]], items[k - 2][1])
                                if 0 <= k - 3 < nit:
                                    sb_s5(ctxs[items[k - 3][0]], items[k - 3][1], 2 * pair + items[k - 3][0])
                            for c in range(2):
                                h = 2 * pair + c
                                S.dma("sp", ODS[c], obT[0 * 512 + h * 128:0 * 512 + (h + 1) * 128, :], ctxs[c]["oT"][:], reads=[ctxs[c]["Bo"]])
                        S.barrier()


                if "B" in stages:
                    with ExitStack() as bs:
                        vh = sb("b_vh", [128, NT, 128], BF16, bs)
                        Bvh = Buf("b_vh")
                        cn = sb("b_cn", [128, 4, T], BF16, bs)
                        Bcn = Buf("b_cn")
                        kpe = sb("b_kpe", [128, T], BF16, bs)
                        Bkpe = Buf("b_kpe")
                        qk = [(sb("b_q%d" % i, [128, T], BF16, bs), Buf("b_q%d" % i), sb("b_k%d" % i, [128, T], BF16, bs), Buf("b_k%d" % i)) for i in range(1)] * 2
                        qpes = [(sb("b_qpe%d" % i, [128, T], BF16, bs), Buf("b_qpe%d" % i)) for i in range(1)] * 2
                        oTb = [(sb("b_o%d" % i, [128, T], BF16, bs), Buf("b_o%d" % i)) for i in range(1)] * 2
                        cosM = sb("b_cos", [128, T], BF16, bs)
                        sinM = sb("b_sin", [128, T], BF16, bs)
                        S.dma("sp", misc_ds, cosM[:], tabD[0], writes=[Btab])
                        S.dma("sp", misc_ds, sinM[:], tabD[1], writes=[Btab])
                        mctx = []
                        for c in range(2):
                            mctx.append(dict(
                                zc=sb("b_zc%d" % c, [128, T], F32, bs), Bzc=Buf("b_zc%d" % c),
                                w=sb("b_w%d" % c, [128, T], BF16, bs), Bw=Buf("b_w%d" % c),
                                wT=sb("b_wT%d" % c, [128, T], BF16, bs), BwT=Buf("b_wT%d" % c),
                                col=sb("b_col%d" % c, [128, 4], F32, bs), Bcol=Buf("b_col%d" % c),
                                dg=sb("b_dg%d" % c, [128, 128], BF16, bs), Bdg=Buf("b_dg%d" % c),
                                zb=(0, 1) if c == 0 else (4, 5), tb=2 if c == 0 else 6, obk=3 if c == 0 else 7))
                        cf = sb("b_cf", [128, 4, 512], BF16, bs)
                        Bcf = Buf("b_cf")
                        sq = [sb("b_sq%d" % i, [128, 512], BF16, bs) for i in range(2)]
                        Bsq = [Buf("b_sq%d" % i) for i in range(2)]
                        rsb = sb("b_rs", [128, 512], F32, bs)
                        Brs = Buf("b_rs")
                        rt = (sb("b_xs", [128, 512], BF16, bs), sb("b_t1", [128, 512], F32, bs), sb("b_t2", [128, 512], F32, bs),
                              Buf("b_xs"), Buf("b_t1"), Buf("b_t2"))
                        jobs = [wjob("ckv0"), wjob("ckv1")]
                        lin_nb[0] = 3
                        for tg in range(4):
                            bsum = 3
                            for c in range(4):
                                t, b, ncj = jobs[c // 2]

                                def epi_c(tg_, bk, c=c):
                                    S.op("act", lambda e: e.activation(sq[c % 2][:], ps[bk][:, :], AF.Square), [PB[bk]], [Bsq[c % 2]])
                                    S.op("dve", lambda e: e.tensor_copy(cf[:, c, :], ps[bk][:, :]), [PB[bk]], [Bcf])
                                lin_fm(t, b, (c % 2) * 128, 128, KC, h_rhs, [Bh], epi_c, ncj, tgs=[tg])
                                mm(ps[bsum][:, :], ones_bf[:, 0:128], sq[c % 2][:], c == 0, c == 3, [Bones, Bsq[c % 2]], [PB[bsum]], inc=True)
                            S.op("act", lambda e: e.activation(rsb[:], ps[bsum][:, :], AF.Sqrt, bias=ccs[:, CC_EPS:CC_EPS + 1], scale=1.0 / 512.0),
                                 [PB[bsum], Bcc], [Brs])
                            S.op("dve", lambda e: e.reciprocal(rsb[:], rsb[:]), [Brs], [Brs])
                            for c in range(4):
                                S.op("dve", lambda e: e.scalar_tensor_tensor(cn[:, c, tg * 512:(tg + 1) * 512], cf[:, c, :],
                                                                              pvs[:, PV_KVG + c:PV_KVG + c + 1], rsb[:], ALU.mult, ALU.mult),
                                     [Bcf, Brs, Bpv], [Bcn])
                        lin_nb[0] = 4
                        t, b, ncj = wjob("kr")

                        def epi_kr(tg, bk):
                            rope_fm(bk, 64, cosM, sinM, r64_bf, 1.0, kpe[0:64, tg * 512:(tg + 1) * 512], Bkpe, tg, rt)
                        lin_fm(t, b, 0, 64, KC, h_rhs, [Bh], epi_kr, ncj)
                        def cn_rhs(kc, tg):
                            return cn[:, kc, tg * 512:(tg + 1) * 512]

                        def cn_lhs(kc, tile):
                            return cn[:, kc, tile * 128:(tile + 1) * 128]
                        sc = 192.0 ** -0.5

                        def m_s1(cx, i, qT, Bq, kT, Bk, qpe, Bqpe):
                            L = 128 * (i + 1)
                            nch = (L + 511) // 512
                            for ch in range(nch):
                                cw = min(512, L - ch * 512)
                                bk = cx["zb"][ch % 2]
                                cs = slice(ch * 512, ch * 512 + cw)
                                mm(ps[bk][:, 0:cw], qT[:, i * 128:(i + 1) * 128], kT[:, cs], True, False, [Bq, Bk], [PB[bk]])
                                mm(ps[bk][:, 0:cw], qpe[0:64, i * 128:(i + 1) * 128], kpe[0:64, cs], False, True, [Bqpe, Bkpe], [PB[bk]])
                                evac(cx["zc"][:, cs], ps[bk][:, 0:cw], [PB[bk]], [cx["Bzc"]])
                            dsl = slice(L - 128, L)
                            S.op("dve", lambda e: e.tensor_tensor(cx["zc"][:, dsl], cx["zc"][:, dsl], cmf[:, 0, :], ALU.add), [cx["Bzc"], Bcm], [cx["Bzc"]])

                        def m_s2(cx, i):
                            L = 128 * (i + 1)
                            col, Bcol = cx["col"], cx["Bcol"]
                            S.op("dve", lambda e: e.tensor_reduce(col[:, 0:1], cx["zc"][:, 0:L], AX.X, ALU.max, negate=True), [cx["Bzc"]], [Bcol])
                            S.op("act", lambda e: e.activation(cx["w"][:, 0:L], cx["zc"][:, 0:L], AF.Exp, bias=col[:, 0:1], accum_out=col[:, 1:2]),
                                 [cx["Bzc"], Bcol], [cx["Bw"], Bcol])
                            S.op("dve", lambda e: e.reciprocal(col[:, 2:3], col[:, 1:2]), [Bcol], [Bcol])
                            S.op("dve", lambda e: e.tensor_scalar(cx["dg"][:], ident_f[:], col[:, 2:3], None, ALU.mult), [Bcm, Bcol], [cx["Bdg"]])

                        def m_s3(cx, i):
                            kb = 0
                            tb = cx["tb"]
                            while kb <= i:
                                n4 = min(4, i + 1 - kb)
                                for j in range(n4):
                                    S.op("pe", lambda e: e.matmul(ps[tb][:, j * 128:(j + 1) * 128], cx["w"][:, (kb + j) * 128:(kb + j + 1) * 128], cx["dg"][:],
                                                                  start=True, stop=True), [cx["Bw"], cx["Bdg"]], [PB[tb]], inc=(j == n4 - 1))
                                evac(cx["wT"][:, kb * 128:(kb + n4) * 128], ps[tb][:, 0:n4 * 128], [PB[tb]], [cx["BwT"]])
                                kb += n4

                        def m_s4(cx, i, oT, Bo):
                            ok = cx["obk"]
                            for kb in range(i + 1):
                                mm(ps[ok][:, 0:128], vh[:, kb, :], cx["wT"][:, kb * 128:(kb + 1) * 128], kb == 0, kb == i, [Bvh, cx["BwT"]], [PB[ok]])
                            evac(oT[:, i * 128:(i + 1) * 128], ps[ok][:, 0:128], [PB[ok]], [Bo])

                        for h in range(4):
                            qT, Bq, kT, Bk = qk[h % 2]
                            qpe, Bqpe = qpes[h % 2]
                            oT, Bo = oTb[h % 2]
                            t, b, ncj = wjob("mq%d" % h)

                            def epi_q(tg, bk, qT=qT, Bq=Bq):
                                evac(qT[:, tg * 512:(tg + 1) * 512], ps[bk][:, :], [PB[bk]], [Bq], scale=sc)

                            def epi_qpe(tg, bk, qpe=qpe, Bqpe=Bqpe):
                                rope_fm(bk, 64, cosM, sinM, r64_bf, sc, qpe[0:64, tg * 512:(tg + 1) * 512], Bqpe, tg, rt)
                            lin_fm(t, b, 0, 128, KC, h_rhs, [Bh], epi_q, ncj)
                            lin_fm(t, b, 128, 64, KC, h_rhs, [Bh], epi_qpe, ncj)
                            t, b = wload(ukv[l, :, h * 512:(h + 1) * 512], 512)

                            def epi_k(tg, bk, kT=kT, Bk=Bk):
                                evac(kT[:, tg * 512:(tg + 1) * 512], ps[bk][:, :], [PB[bk]], [Bk])
                            lin_fm(t, b, 0, 128, 4, cn_rhs, [Bcn], epi_k, 128)
                            half = h // 2
                            t, b = wload(ukv[l, :, 2048 + half * 1024:2048 + (half + 1) * 1024], 1024)

                            def epi_v(tile, bk):
                                evac(vh[:, tile, :], ps[bk][:, 0:128], [PB[bk]], [Bvh])
                            lin_tm(t, b, (h % 2) * 128, 128, 4, cn_lhs, [Bcn], epi_v, 256)
                            for k in range(NT + 3):
                                if k < NT:
                                    m_s1(mctx[k % 2], k, qT, Bq, kT, Bk, qpe, Bqpe)
                                if 0 <= k - 1 < NT:
                                    m_s2(mctx[(k - 1) % 2], k - 1)
                                if 0 <= k - 2 < NT:
                                    m_s3(mctx[(k - 2) % 2], k - 2)
                                if 0 <= k - 3 < NT:
                                    m_s4(mctx[(k - 3) % 2], k - 3, oT, Bo)
                            S.dma("sp", ODS[h % 2], obT[1 * 512 + h * 128:1 * 512 + (h + 1) * 128, :], oT[:], reads=[Bo])
                        gate_flush()
                        S.barrier()

                if "C" in stages:
                    with ExitStack() as bs:
                        vall = sb("c_v", [128, NT, 512], BF16, bs)
                        Bv = Buf("c_v")
                        sg = sb("c_sg", [128, NT, 512], BF16, bs)
                        Bsg = Buf("c_sg")
                        rt = (sb("c_xs", [128, 512], BF16, bs), sb("c_t1", [128, 512], F32, bs), sb("c_t2", [128, 512], F32, bs),
                              Buf("c_xs"), Buf("c_t1"), Buf("c_t2"))
                        cctx = []
                        for c in range(2):
                            cx = dict(
                                qr=sb("c_qr%d" % c, [128, T], BF16, bs), Bqr=Buf("c_qr%d" % c),
                                kr=sb("c_kr%d" % c, [128, T], BF16, bs), Bkr=Buf("c_kr%d" % c),
                                qd=sb("c_qd%d" % c, [128, T], BF16, bs), Bqd=Buf("c_qd%d" % c),
                                oT=sb("c_o%d" % c, [128, T], BF16, bs), Bo=Buf("c_o%d" % c),
                                stf=sb("c_stf%d" % c, [128, 128], F32, bs), Bstf=Buf("c_stf%d" % c),
                                stb=[(sb("c_stb%d_%d" % (c, i), [128, 128], BF16, bs), Buf("c_stb%d_%d" % (c, i))) for i in range(2)],
                                sTm=[(sb("c_sTm%d_%d" % (c, i), [128, 128], BF16, bs), Buf("c_sTm%d_%d" % (c, i))) for i in range(2)],
                                kd=[(sb("c_kd%d_%d" % (c, i), [128, 128], BF16, bs), Buf("c_kd%d_%d" % (c, i))) for i in range(2)],
                                og=[(sb("c_og%d_%d" % (c, i), [128, 128], BF16, bs), Buf("c_og%d_%d" % (c, i))) for i in range(2)],
                                junk=sb("c_junk%d" % c, [128, 128], F32, bs), Bjunk=Buf("c_junk%d" % c),
                                col=sb("c_col%d" % c, [128, 4], F32, bs), Bcol=Buf("c_col%d" % c),
                                pb=(0, 1, 2, 3) if c == 0 else (4, 5, 6, 7))
                            cctx.append(cx)
                        cosR = sb("c_cos", [128, T], BF16, bs)
                        sinR = sb("c_sin", [128, T], BF16, bs)
                        S.dma("sp", misc_ds, cosR[:], tabD[2], writes=[Btab])
                        S.dma("sp", misc_ds, sinR[:], tabD[3], writes=[Btab])
                        for half in range(2):
                            t, b, ncj = wjob("rv%d" % half)

                            def epi_v(tile, bk, half=half):
                                evac(vall[:, tile, half * 256:(half + 1) * 256], ps[bk][:, 0:256], [PB[bk]], [Bv])
                            lin_tm(t, b, 0, 256, KC, h_lhs, [Bh], epi_v, ncj)
                        for half in range(2):
                            t, b, ncj = wjob("rg%d" % half)

                            def epi_g(tile, bk, half=half):
                                S.op("act", lambda e: e.activation(sg[:, tile, half * 256:(half + 1) * 256], ps[bk][:, 0:256], AF.Silu), [PB[bk]], [Bsg])
                            lin_tm(t, b, 0, 256, KC, h_lhs, [Bh], epi_g, ncj)

                        def c_s1(cx, h, n):
                            csl = slice(n * 128, (n + 1) * 128)
                            sT_, BsT = cx["sTm"][n % 2]
                            b0 = cx["pb"][0]
                            mm(ps[b0][:, 0:128], cx["kr"][:, csl], cx["qr"][:, csl], True, True, [cx["Bkr"], cx["Bqr"]], [PB[b0]])
                            S.op("dve", lambda e: e.tensor_tensor(sT_[:], ps[b0][:, 0:128], cmf[:, 1 + h, :], ALU.mult), [PB[b0], Bcm], [BsT])

                        def c_s2(cx, h, n):
                            csl = slice(n * 128, (n + 1) * 128)
                            hs = slice(h * 128, (h + 1) * 128)
                            sT_, BsT = cx["sTm"][n % 2]
                            og_, Bog = cx["og"][n % 2]
                            b1 = cx["pb"][1]
                            col, Bcol = cx["col"], cx["Bcol"]
                            mm(ps[b1][:, 0:128], sT_[:], vall[:, n, hs], True, n == 0, [BsT, Bv], [PB[b1]])
                            if n > 0:
                                sb_, Bsb = cx["stb"][n % 2]
                                mm(ps[b1][:, 0:128], cx["qd"][:, csl], sb_[:], False, True, [cx["Bqd"], Bsb], [PB[b1]])
                            S.op("act", lambda e: e.activation(cx["junk"][:], ps[b1][:, 0:128], AF.Square, accum_out=col[:, 0:1]), [PB[b1]], [cx["Bjunk"], Bcol])
                            S.op("act", lambda e: e.activation(col[:, 1:2], col[:, 0:1], AF.Sqrt, bias=ccs[:, CC_EPS:CC_EPS + 1], scale=1.0 / 128.0),
                                 [Bcol, Bcc], [Bcol])
                            S.op("dve", lambda e: e.reciprocal(col[:, 2:3], col[:, 1:2]), [Bcol], [Bcol])
                            S.op("dve", lambda e: e.scalar_tensor_tensor(og_[:], ps[b1][:, 0:128], col[:, 2:3], sg[:, n, hs], ALU.mult, ALU.mult),
                                 [PB[b1], Bcol, Bsg], [Bog])

                        def c_s3(cx, h, n):
                            csl = slice(n * 128, (n + 1) * 128)
                            og_, Bog = cx["og"][n % 2]
                            b2 = cx["pb"][2]
                            mm(ps[b2][:, 0:128], og_[:], ident_bf, True, True, [Bog, Bcm], [PB[b2]])
                            evac(cx["oT"][:, csl], ps[b2][:, 0:128], [PB[b2]], [cx["Bo"]])

                        def c_s4(cx, h, n):
                            if n >= NT - 1:
                                return
                            csl = slice(n * 128, (n + 1) * 128)
                            hs = slice(h * 128, (h + 1) * 128)
                            kd_, Bkd = cx["kd"][n % 2]
                            b3 = cx["pb"][3]
                            stf, Bstf = cx["stf"], cx["Bstf"]
                            mm(ps[b3][:, 0:128], cx["kr"][:, csl], ident_bf, True, True, [cx["Bkr"], Bcm], [PB[b3]])
                            S.op("act", lambda e: e.activation(kd_[:], ps[b3][:, 0:128], AF.Copy, scale=ccs[:, CC_KDEC + h:CC_KDEC + h + 1]),
                                 [PB[b3], Bcc], [Bkd])
                            mm(ps[b3][:, 128:256], kd_[:], vall[:, n, hs], True, True, [Bkd, Bv], [PB[b3]])
                            if n == 0:
                                S.op("dve", lambda e: e.tensor_copy(stf[:], ps[b3][:, 128:256]), [PB[b3]], [Bstf])
                            else:
                                S.op("dve", lambda e: e.scalar_tensor_tensor(stf[:], stf[:], CD[h], ps[b3][:, 128:256], ALU.mult, ALU.add),
                                     [Bstf, PB[b3]], [Bstf])
                            sbn, Bsbn = cx["stb"][(n + 1) % 2]
                            S.op("act", lambda e: e.activation(sbn[:], stf[:], AF.Copy), [Bstf], [Bsbn])

                        for pair in range(2):
                            for c in range(2):
                                h = 2 * pair + c
                                cx = cctx[c]
                                t, b, ncj = wjob("rqk%d" % h)

                                def epi_q(tg, bk, cx=cx):
                                    rope_fm(bk, 128, cosR, sinR, r128_bf, 1.0, cx["qr"][:, tg * 512:(tg + 1) * 512], cx["Bqr"], tg, rt)

                                def epi_k(tg, bk, cx=cx):
                                    rope_fm(bk, 128, cosR, sinR, r128_bf, 128.0 ** -0.5, cx["kr"][:, tg * 512:(tg + 1) * 512], cx["Bkr"], tg, rt)
                                lin_fm(t, b, 0, 128, KC, h_rhs, [Bh], epi_q, ncj)
                                lin_fm(t, b, 128, 128, KC, h_rhs, [Bh], epi_k, ncj)
                                for n in range(NT):
                                    csl = slice(n * 128, (n + 1) * 128)
                                    S.op("dve", lambda e: e.tensor_tensor(cx["qd"][:, csl], cx["qr"][:, csl], cmf[:, 5 + h, :], ALU.mult), [cx["Bqr"], Bcm], [cx["Bqd"]])
                            for n in range(NT):
                                for stg in (c_s1, c_s2, c_s3, c_s4):
                                    for c in range(2):
                                        stg(cctx[c], 2 * pair + c, n)
                            for c in range(2):
                                h = 2 * pair + c
                                S.dma("sp", ODS[c], obT[2 * 512 + h * 128:2 * 512 + (h + 1) * 128, :], cctx[c]["oT"][:], reads=[cctx[c]["Bo"]])
                        gate_flush()
                        S.barrier()


                if "D" in stages:
                    with ExitStack() as bs:
                        names = ["xf", "yf", "xc", "r", "ig", "a", "bb", "hh"]
                        Lb = {n: sb("d_" + n, [128, T], F32, bs) for n in names}
                        LB = {n: Buf("d_" + n) for n in names}
                        xcb = sb("d_xcb", [128, T], BF16, bs)
                        Bxcb = Buf("d_xcb")
                        oT = sb("d_o", [128, T], BF16, bs)
                        Bo = Buf("d_o")
                        waxs = sb("d_wax", [128, 1024], BF16, bs)
                        Bwax = Buf("d_wax")
                        nsp = sb("d_nsp", [128, 8], F32, bs)
                        Bnsp = Buf("d_nsp")
                        wl_n[0] = 2
                        S.barrier(only=("pool",))
                        S.dma("pool", misc_ds, waxs[:], wax[l], writes=[Bwax])
                        lin_nb[0] = 3
                        gbanks[0] = [3, 4, 5, 6, 7]
                        hk = [0]

                        def d_hook():
                            hk[0] += 1
                            tick(1 + (hk[0] % 2))
                        S.hook = d_hook
                        S.op("act", lambda e: e.activation(nsp[:, 0:4], pvs[:, PV_LAM:PV_LAM + 4], AF.Exp, scale=-1.0), [Bpv], [Bnsp])
                        S.op("act", lambda e: e.activation(nsp[:, 0:4], nsp[:, 0:4], AF.Ln, bias=ccs[:, CC_ONE:CC_ONE + 1]), [Bnsp, Bcc], [Bnsp])
                        S.op("dve", lambda e: e.tensor_scalar(nsp[:, 4:8], nsp[:, 0:4], -16.0, None, ALU.mult), [Bnsp], [Bnsp])
                        S.op("dve", lambda e: e.tensor_scalar(nsp[:, 0:4], nsp[:, 0:4], -8.0, None, ALU.mult), [Bnsp], [Bnsp])
                        for c in range(4):
                            xf, yf, xc, r_, ig, a_, bb, hh = [Lb[n] for n in names]
                            t, b, ncj = wjob("lxy%d" % c)

                            def epi_x(tg, bk):
                                evac(xf[:, tg * 512:(tg + 1) * 512], ps[bk][:, :], [PB[bk]], [LB["xf"]])

                            def epi_y(tg, bk):
                                evac(yf[:, tg * 512:(tg + 1) * 512], ps[bk][:, :], [PB[bk]], [LB["yf"]])
                            lin_fm(t, b, 0, 128, KC, h_rhs, [Bh], epi_x, ncj)
                            lin_fm(t, b, 128, 128, KC, h_rhs, [Bh], epi_y, ncj)

                            def cwc(tap):
                                return pvs[:, PV_CW + 4 * tap + c:PV_CW + 4 * tap + c + 1]
                            S.op("dve", lambda e: e.tensor_scalar(xc[:], xf[:], cwc(3), pvs[:, PV_CB + c:PV_CB + c + 1], ALU.mult, ALU.add),
                                 [LB["xf"], Bpv], [LB["xc"]])
                            for tap, sh in ((2, 1), (1, 2), (0, 3)):
                                S.op("dve", lambda e: e.scalar_tensor_tensor(xc[:, sh:T], xf[:, 0:T - sh], cwc(tap), xc[:, sh:T], ALU.mult, ALU.add),
                                     [LB["xf"], LB["xc"], Bpv], [LB["xc"]])
                            S.op("act", lambda e: e.activation(xcb[:], xc[:], AF.Copy), [LB["xc"]], [Bxcb])
                            for tg in range(4):
                                sl = slice(tg * 512, (tg + 1) * 512)
                                bk = lin_bank()
                                mm(ps[bk][:, :], waxs[:, c * 128:(c + 1) * 128], xcb[:, sl], True, True, [Bwax, Bxcb], [PB[bk]])
                                S.op("act", lambda e: e.activation(r_[:, sl], ps[bk][:, :], AF.Sigmoid, bias=pvs[:, PV_BA + c:PV_BA + c + 1]),
                                     [PB[bk], Bpv], [LB["r"]])
                                bk = lin_bank()
                                mm(ps[bk][:, :], waxs[:, 512 + c * 128:512 + (c + 1) * 128], xcb[:, sl], True, True, [Bwax, Bxcb], [PB[bk]])
                                S.op("act", lambda e: e.activation(ig[:, sl], ps[bk][:, :], AF.Sigmoid, bias=pvs[:, PV_BX + c:PV_BX + c + 1]),
                                     [PB[bk], Bpv], [LB["ig"]])
                            S.op("act", lambda e: e.activation(a_[:], r_[:], AF.Exp, scale=nsp[:, c:c + 1]), [LB["r"], Bnsp], [LB["a"]])
                            S.op("act", lambda e: e.activation(bb[:], r_[:], AF.Exp, scale=nsp[:, 4 + c:5 + c]), [LB["r"], Bnsp], [LB["bb"]])
                            S.op("dve", lambda e: e.tensor_scalar(bb[:], bb[:], -1.0, 1.0, ALU.mult, ALU.add), [LB["bb"]], [LB["bb"]])
                            S.op("dve", lambda e: e.tensor_scalar(bb[:], bb[:], 0.0, None, ALU.max), [LB["bb"]], [LB["bb"]])
                            S.op("act", lambda e: e.activation(bb[:], bb[:], AF.Sqrt), [LB["bb"]], [LB["bb"]])
                            S.op("dve", lambda e: e.tensor_tensor(ig[:], ig[:], xc[:], ALU.mult), [LB["ig"], LB["xc"]], [LB["ig"]])
                            S.op("dve", lambda e: e.tensor_tensor(bb[:], bb[:], ig[:], ALU.mult), [LB["bb"], LB["ig"]], [LB["bb"]])
                            S.op("dve", lambda e: e.tensor_tensor_scan(hh[:], a_[:], bb[:], 0.0, ALU.mult, ALU.add), [LB["a"], LB["bb"]], [LB["hh"]])
                            S.op("dve", lambda e: e.tensor_tensor(r_[:], yf[:], yf[:], ALU.mult), [LB["yf"]], [LB["r"]])
                            S.op("dve", lambda e: e.tensor_scalar(r_[:], r_[:], 0.044715, 1.0, ALU.mult, ALU.add), [LB["r"]], [LB["r"]])
                            S.op("dve", lambda e: e.tensor_tensor(r_[:], r_[:], yf[:], ALU.mult), [LB["r"], LB["yf"]], [LB["r"]])
                            S.op("act", lambda e: e.activation(r_[:], r_[:], AF.Sigmoid, scale=2.0 * math.sqrt(2.0 / math.pi)), [LB["r"]], [LB["r"]])
                            S.op("dve", lambda e: e.tensor_tensor(r_[:], r_[:], yf[:], ALU.mult), [LB["r"], LB["yf"]], [LB["r"]])
                            S.op("dve", lambda e: e.tensor_tensor(oT[:], hh[:], r_[:], ALU.mult), [LB["hh"], LB["r"]], [Bo])
                            S.dma("sp", ODS[c % 2], obT[3 * 512 + c * 128:3 * 512 + (c + 1) * 128, :], oT[:], reads=[Bo])
                        S.hook = None
                        gate_flush()
                        lin_nb[0] = 4
                        S.barrier()

                if "gate" in stages:
                    gate_flush()
                    gbanks[0] = [0, 1, 2, 3]
                    for _ in ggen:
                        pass
                    gate_flush()
                    lin_nb[0] = 4
                    S.barrier()
                    with ExitStack() as bs:
                        ob = sb("g_ob", [128, 16, T], BF16, bs)
                        Bob = Buf("g_ob")
                        acc = sb("g_acc", [128, T], F32, bs)
                        Bacc = Buf("g_acc")
                        tmp4 = [(sb("g_tmp%d" % i, [128, T], F32, bs), Buf("g_tmp%d" % i)) for i in range(1)] * 2
                        mixc = sb("g_mix", [128, T], BF16, bs)
                        Bmix = Buf("g_mix")
                        wbs = [(sb("g_wb%d" % i, [128, 2048], BF16, bs), Buf("g_wb%d" % i)) for i in range(2)]
                        sgm = [(hbuf[:, 4 * i:4 * i + 4, :], Buf("g_sgm%d" % i)) for i in range(2)]
                        obv = obT.rearrange("(c p) t -> p c t", p=128)
                        for q4 in range(4):
                            S.dma("sp", misc_ds, ob[:, q4 * 4:(q4 + 1) * 4, :], obv[:, q4 * 4:(q4 + 1) * 4, :], writes=[Bob])
                        ti = 0
                        S.barrier(only=("pool",))
                        def load_sg(mm_):
                            S.dma("sp", XDS[mm_ % 2], sgm[mm_ % 2][0], sgD[:, mm_ * 128:(mm_ + 1) * 128, :].rearrange("b p t -> p b t"),
                                  writes=[sgm[mm_ % 2][1]])
                        load_sg(0)
                        for m in range(16):
                            sg_t, sg_b = sgm[m % 2]
                            if m + 1 < 16:
                                load_sg(m + 1)
                            wb_t, wb_b = wbs[m % 2]
                            S.dma("pool", WBDS[m % 2], wb_t[:], wbr[l, m], writes=[wb_b])
                            tbv = wb_t[:, :].rearrange("p (b c j) -> p b c j", b=4, c=4)
                            for b in range(4):
                                tmp, Btmp = tmp4[ti % 2]
                                ti += 1
                                for tg in range(4):
                                    sl = slice(tg * 512, (tg + 1) * 512)
                                    bkp = lin_bank()
                                    for kc in range(4):
                                        mm(ps[bkp][:, :], tbv[:, b, kc, :], ob[:, b * 4 + kc, sl], kc == 0, kc == 3, [wb_b, Bob], [PB[bkp]])
                                    if b == 0:
                                        S.op("dve", lambda e: e.tensor_tensor(acc[:, sl], sg_t[:, b, sl], ps[bkp][:, :], ALU.mult), [sg_b, PB[bkp]], [Bacc])
                                    else:
                                        S.op("dve", lambda e: e.tensor_tensor(tmp[:, sl], sg_t[:, b, sl], ps[bkp][:, :], ALU.mult), [sg_b, PB[bkp]], [Btmp])
                                if 0 < b < 3:
                                    S.op("dve", lambda e: e.tensor_tensor(acc[:], acc[:], tmp[:], ALU.add), [Bacc, Btmp], [Bacc])
                                elif b == 3:
                                    S.op("dve", lambda e: e.tensor_tensor(mixc[:], acc[:], tmp[:], ALU.add), [Bacc, Btmp], [Bmix])
                            S.dma("sp", ODS[m % 2], mixT[m * 128:(m + 1) * 128, :], mixc[:], reads=[Bmix])
                        S.barrier()
                    wl_n[0] = 3


                if "wout" in stages:
                    with ExitStack() as bs:
                        xt = [(sb("o_x%d" % i, [128, T], F32, bs), Buf("o_x%d" % i)) for i in range(2)]
                        xds = XDS
                        mv = mixT.rearrange("(c p) t -> p c t", p=128)
                        for q4 in range(4):
                            S.dma("sp", misc_ds, hbuf[:, q4 * 4:(q4 + 1) * 4, :], mv[:, q4 * 4:(q4 + 1) * 4, :], writes=[Bh])
                        def load_x(mm_):
                            S.dma("sp", xds[mm_ % 2], xt[mm_ % 2][0][:], (xT_in if l == 0 else xres)[mm_ * 128:(mm_ + 1) * 128, :], writes=[xt[mm_ % 2][1]])
                        load_x(0)
                        for m in range(16):
                            x_, Bx_ = xt[m % 2]
                            if m + 1 < 16:
                                load_x(m + 1)
                            t, b = wload(wout[l, m], 2048)
                            wv = t[:, 0:2048].rearrange("p (c j) -> p c j", j=128)
                            for tg in range(4):
                                sl = slice(tg * 512, (tg + 1) * 512)
                                bk = lin_bank()
                                for kc in range(KC):
                                    mm(ps[bk][:, :], wv[:, kc, :], hbuf[:, kc, sl], kc == 0, kc == KC - 1, [b, Bh], [PB[bk]])
                                S.op("dve", lambda e: e.tensor_tensor(x_[:, sl], x_[:, sl], ps[bk][:, :], ALU.add), [Bx_, PB[bk]], [Bx_])
                            S.dma("sp", xds[m % 2], xres[m * 128:(m + 1) * 128, :], x_[:], reads=[Bx_])
                        S.barrier()

            if "ffn" in stages:
                norm_phase(xres, pvs, PV_LN2, False)
                with ExitStack() as bs:
                    aT = sb("f_a", [128, FC, 512], BF16, bs)
                    Ba = Buf("f_a")
                    sgf = [(sb("f_sg%d" % i, [128, 512], F32, bs), Buf("f_sg%d" % i)) for i in range(2)]
                    xq = [(sb("f_x%d" % i, [128, 512], F32, bs), Buf("f_x%d" % i)) for i in range(2)]
                    fds = FDS
                    for tq in range(4):
                        sl = slice(tq * 512, (tq + 1) * 512)
                        for f in range(FC):
                            t, b = wload(wfgu[l, f], 4096)
                            wv = t[:, 0:4096].rearrange("p (g c j) -> p g c j", g=2, c=KC)
                            bkg = lin_bank()
                            for kc in range(KC):
                                mm(ps[bkg][:, :], wv[:, 0, kc, :], hbuf[:, kc, sl], kc == 0, kc == KC - 1, [b, Bh], [PB[bkg]])
                            bku = lin_bank()
                            for kc in range(KC):
                                mm(ps[bku][:, :], wv[:, 1, kc, :], hbuf[:, kc, sl], kc == 0, kc == KC - 1, [b, Bh], [PB[bku]])
                            sg_, Bsg_ = sgf[f % 2]
                            S.op("act", lambda e: e.activation(sg_[:], ps[bkg][:, :], AF.Silu), [PB[bkg]], [Bsg_])
                            S.op("dve", lambda e: e.tensor_tensor(aT[:, f, :], sg_[:], ps[bku][:, :], ALU.mult), [Bsg_, PB[bku]], [Ba])
                        def load_xq(mm_, sl=sl):
                            S.dma("sp", fds[mm_ % 2], xq[mm_ % 2][0][:], xres[mm_ * 128:(mm_ + 1) * 128, sl], writes=[xq[mm_ % 2][1]])
                        load_xq(0)
                        for m in range(16):
                            x_, Bx_ = xq[m % 2]
                            if m + 1 < 16:
                                load_xq(m + 1)
                            t, b = wload(wfd[l, m], FC * 128)
                            wv = t[:, 0:FC * 128].rearrange("p (c j) -> p c j", j=128)
                            bk = lin_bank()
                            for f in range(FC):
                                mm(ps[bk][:, :], wv[:, f, :], aT[:, f, :], f == 0, f == FC - 1, [b, Ba], [PB[bk]])
                            S.op("dve", lambda e: e.tensor_tensor(x_[:], x_[:], ps[bk][:, :], ALU.add), [Bx_, PB[bk]], [Bx_])
                            S.dma("sp", fds[m % 2], xres[m * 128:(m + 1) * 128, sl], x_[:], reads=[Bx_])
                    S.barrier()

        if final_norm:
            norm_phase(xres, lnfs, 0, True)
        S.barrier(skip=())
        for ds in S.dsems:
            if ds.cnt > 0:
                nc.sync.wait_ge(ds.sem, ds.cnt)
        build._ninst = S.ninst
        build._stuck = S.check_deadlock()
    return nc


def _common_inputs(inp):
    cc, cm, _ = _consts()
    return {"cc": cc, "cm": cm, "lnf": _col128(np.asarray(inp["ln_final"], np.float32))}


def kernel(**inputs):
    inp = {k: np.asarray(v) for k, v in inputs.items()}
    n = 8
    nc = build(DEPTH, final_norm=True)
    W = prep_weights(inp, list(range(DEPTH)))
    com = _common_inputs(inp)
    in_maps = []
    for c in range(n):
        m = {"xT": np.ascontiguousarray(inp["x"][c].T),
             "pos": np.ascontiguousarray(inp["positions"][c:c + 1].astype(np.int32))}
        m.update(com)
        m.update(W)
        in_maps.append(m)
    res = run_bass_kernel_spmd(nc, in_maps, core_ids=list(range(n)))
    out = np.stack([np.ascontiguousarray(res.results[c]["outT"].T) for c in range(n)], axis=0)
    return out.astype(np.float32)
```
